# Optimizing a Trainium2 kernel written in Bass

```python
import math
import jax
import jax.numpy as jnp
from jax import lax
import numpy as np

D_MODEL = 2048
BATCH = 4
SEQ = 8192
DEPTH = 4

A_HEADS = 4
A_QK_DIM = 64
A_V_DIM = 128
B_HEADS = 4
B_Q_LORA = 512
B_KV_LORA = 256
B_NOPE = 128
B_ROPE = 64
B_V_DIM = 128
ROPE_THETA = 10000.0
C_HEADS = 16
C_KV_HEADS = 4
C_GROUP = C_HEADS // C_KV_HEADS
C_HEAD_DIM = 128
WINDOW = 128
BLOCK = 128
D_FF = 5632
N_EXPERTS = 8
TOP_K = 2
EXPERT_FF = 2816
MOE_BLOCK = 512
ALPHA = (2 * DEPTH) ** 0.25
BETA = (8 * DEPTH) ** -0.25
LN_EPS = 1e-5
RMS_EPS = 1e-6
ADA_STD = 0.5
N_EVEN = (DEPTH + 1) // 2
N_ODD = DEPTH // 2

A_Q_COLS = A_HEADS * 2 * A_QK_DIM
A_K_COLS = A_HEADS * 2 * A_QK_DIM
A_V_COLS = A_HEADS * A_V_DIM
AB_SPLITS = (A_Q_COLS, A_K_COLS, A_V_COLS, B_Q_LORA, B_KV_LORA, B_ROPE)
AB_IN_COLS = A_Q_COLS + A_K_COLS + A_V_COLS + B_Q_LORA + B_KV_LORA + B_ROPE
AB_OUT_IN = A_HEADS * A_V_DIM + B_HEADS * B_V_DIM
C_Q_COLS = C_HEADS * C_HEAD_DIM
C_KV_COLS = C_KV_HEADS * C_HEAD_DIM
C_SPLITS = (C_Q_COLS, C_KV_COLS, C_KV_COLS)
C_IN_COLS = C_Q_COLS + 2 * C_KV_COLS
C_OUT_IN = C_Q_COLS

kernel_name = 'hybrid_diff_mla_swa_moe_encoder'


def split_cols(t, sizes):
    idx = np.cumsum(sizes)[:-1].tolist()
    return jnp.split(t, idx, axis=-1)


def layer_norm(x, g, b):
    xf = x.astype(jnp.float32)
    mu = jnp.mean(xf, axis=-1, keepdims=True)
    var = jnp.mean(jnp.square(xf - mu), axis=-1, keepdims=True)
    return ((xf - mu) * lax.rsqrt(var + LN_EPS)).astype(x.dtype) * g + b


def rms_norm(x, g):
    xf = x.astype(jnp.float32)
    return (xf * lax.rsqrt(jnp.mean(xf * xf, axis=-1, keepdims=True) + RMS_EPS)).astype(x.dtype) * g


def alibi_slopes(n_heads):
    return jnp.asarray(2.0 ** (-8.0 * np.arange(1, n_heads + 1) / n_heads), dtype=jnp.float32)


def rope_tables(seq):
    inv_freq = ROPE_THETA ** (-jnp.arange(0, B_ROPE, 2, dtype=jnp.float32) / B_ROPE)
    ang = jnp.arange(seq, dtype=jnp.float32)[:, None] * inv_freq[None, :]
    return jnp.cos(ang), jnp.sin(ang)


def apply_rope(x, cos, sin):
    x1, x2 = jnp.split(x, 2, axis=-1)
    return jnp.concatenate([x1 * cos - x2 * sin, x2 * cos + x1 * sin], axis=-1).astype(x.dtype)


def sweep_query_blocks(block_fn, *qs):
    bsz, seq = qs[0].shape[:2]
    nb = seq // BLOCK
    blocked = tuple(jnp.swapaxes(q.reshape(bsz, nb, BLOCK, *q.shape[2:]), 0, 1) for q in qs)
    out = lax.map(lambda args: block_fn(*args), (jnp.arange(nb),) + blocked)
    out = jnp.swapaxes(out, 0, 1)
    return out.reshape(bsz, seq, *out.shape[3:])


def mixer_ab(h, w_in, lam, diff_g, q_norm_g, kv_norm_g, w_uq, w_ukv, w_out, lam_init, cos, sin):
    bsz, seq, _ = h.shape
    pos = jnp.arange(seq)
    aq, ak, av, cq, ckv, kr = split_cols(h @ w_in, AB_SPLITS)

    aq = aq.reshape(bsz, seq, A_HEADS, 2, A_QK_DIM)
    ak = ak.reshape(bsz, seq, A_HEADS, 2, A_QK_DIM)
    av = av.reshape(bsz, seq, A_HEADS, A_V_DIM)
    lam32 = lam.astype(jnp.float32)
    lam_full = (jnp.exp(jnp.sum(lam32[0] * lam32[1])) - jnp.exp(jnp.sum(lam32[2] * lam32[3])) + lam_init)
    slopes_a = alibi_slopes(A_HEADS)[:, None, None]
    scale_a = A_QK_DIM ** -0.5

    def diff_block(j, qb):
        t = j * BLOCK + jnp.arange(BLOCK)
        dist = jnp.abs(t[:, None] - pos[None, :]).astype(jnp.float32)
        s = jnp.einsum('bqhmd,bkhmd->bmhqk', qb, ak, preferred_element_type=jnp.float32) * scale_a - slopes_a * dist
        e = jnp.exp(s - jnp.max(s, axis=-1, keepdims=True))
        inv_l = jnp.transpose(1.0 / jnp.sum(e, axis=-1), (0, 3, 1, 2))[..., None]
        o = jnp.einsum('bmhqk,bkhd->bqmhd', e.astype(av.dtype), av, preferred_element_type=jnp.float32) * inv_l
        return (o[:, :, 0] - lam_full * o[:, :, 1]).astype(av.dtype)

    o_a = sweep_query_blocks(diff_block, aq)
    o_a = (rms_norm(o_a, diff_g) * (1.0 - lam_init)).reshape(bsz, seq, A_V_COLS)

    q = (rms_norm(cq, q_norm_g) @ w_uq).reshape(bsz, seq, B_HEADS, B_NOPE + B_ROPE)
    q_nope = q[..., :B_NOPE]
    q_rope = apply_rope(q[..., B_NOPE:], cos[:, None, :], sin[:, None, :])
    kv = (rms_norm(ckv, kv_norm_g) @ w_ukv).reshape(bsz, seq, B_HEADS, B_NOPE + B_V_DIM)
    k_nope = kv[..., :B_NOPE]
    v_b = kv[..., B_NOPE:]
    k_rope = apply_rope(kr, cos, sin)
    scale_b = (B_NOPE + B_ROPE) ** -0.5

    def mla_block(j, qn, qr):
        s = (jnp.einsum('bqhd,bkhd->bhqk', qn, k_nope, preferred_element_type=jnp.float32)
             + jnp.einsum('bqhr,bkr->bhqk', qr, k_rope, preferred_element_type=jnp.float32)) * scale_b
        e = jnp.exp(s - jnp.max(s, axis=-1, keepdims=True))
        inv_l = jnp.transpose(1.0 / jnp.sum(e, axis=-1), (0, 2, 1))[..., None]
        o = jnp.einsum('bhqk,bkhd->bqhd', e.astype(v_b.dtype), v_b, preferred_element_type=jnp.float32) * inv_l
        return o.astype(v_b.dtype)

    o_b = sweep_query_blocks(mla_block, q_nope, q_rope).reshape(bsz, seq, B_HEADS * B_V_DIM)
    return jnp.concatenate([o_a, o_b], axis=-1) @ w_out


def mixer_c(h, w_in, sink, w_out):
    bsz, seq, _ = h.shape
    q, k, v = split_cols(h @ w_in, C_SPLITS)
    q = q.reshape(bsz, seq, C_KV_HEADS, C_GROUP, C_HEAD_DIM)
    k = k.reshape(bsz, seq, C_KV_HEADS, C_HEAD_DIM)
    v = v.reshape(bsz, seq, C_KV_HEADS, C_HEAD_DIM)
    pad = ((0, 0), (BLOCK, BLOCK), (0, 0), (0, 0))
    kp = jnp.pad(k, pad)
    vp = jnp.pad(v, pad)
    slopes = alibi_slopes(C_HEADS).reshape(C_KV_HEADS, C_GROUP)[:, :, None, None]
    sink_l = sink.astype(jnp.float32).reshape(C_KV_HEADS, C_GROUP)[None, :, :, None, None]
    scale = C_HEAD_DIM ** -0.5

    def band_block(j, qb):
        kb = lax.dynamic_slice_in_dim(kp, j * BLOCK, 3 * BLOCK, axis=1)
        vb = lax.dynamic_slice_in_dim(vp, j * BLOCK, 3 * BLOCK, axis=1)
        t = j * BLOCK + jnp.arange(BLOCK)
        s_pos = j * BLOCK - BLOCK + jnp.arange(3 * BLOCK)
        dist = jnp.abs(t[:, None] - s_pos[None, :])
        valid = (dist <= WINDOW) & (s_pos >= 0)[None, :] & (s_pos < seq)[None, :]
        s = (jnp.einsum('bqngd,bknd->bngqk', qb, kb, preferred_element_type=jnp.float32) * scale
             - slopes * dist.astype(jnp.float32))
        s = jnp.where(valid, s, -jnp.inf)
        m = jnp.maximum(jnp.max(s, axis=-1, keepdims=True), sink_l)
        e = jnp.exp(s - m)
        p = e / (jnp.sum(e, axis=-1, keepdims=True) + jnp.exp(sink_l - m))
        return jnp.einsum('bngqk,bknd->bqngd', p.astype(vb.dtype), vb)

    o = sweep_query_blocks(band_block, q).reshape(bsz, seq, C_OUT_IN)
    return o @ w_out


def swiglu(h, w_gate, w_up, w_down):
    return (jax.nn.silu(h @ w_gate) * (h @ w_up)) @ w_down


def moe_swiglu(h, w_router, b_router, w_gate, w_up, w_down):
    bsz, seq, d = h.shape
    xf = h.reshape(-1, d)
    n_assign = xf.shape[0] * TOP_K
    logits = (xf @ w_router).astype(jnp.float32) + b_router.astype(jnp.float32)
    top_logits, top_idx = lax.top_k(logits, TOP_K)
    gates = jax.nn.softmax(top_logits, axis=-1)
    flat_e = top_idx.reshape(-1).astype(jnp.int32)
    flat_g = gates.reshape(-1)
    order = jnp.argsort(flat_e)
    sorted_e = flat_e[order]
    sizes = jnp.bincount(flat_e, length=N_EXPERTS).astype(jnp.int32)
    starts = jnp.cumsum(sizes) - sizes
    padded = ((sizes + MOE_BLOCK - 1) // MOE_BLOCK) * MOE_BLOCK
    pad_end = jnp.cumsum(padded)
    pad_start = pad_end - padded
    dest = pad_start[sorted_e] + (jnp.arange(n_assign, dtype=jnp.int32) - starts[sorted_e])
    n_blocks = -(-n_assign // MOE_BLOCK) + N_EXPERTS
    n_rows = n_blocks * MOE_BLOCK
    row_tok = jnp.zeros((n_rows,), jnp.int32).at[dest].set((order // TOP_K).astype(jnp.int32))
    row_gate = jnp.zeros((n_rows,), flat_g.dtype).at[dest].set(flat_g[order])
    block_start = jnp.arange(n_blocks, dtype=jnp.int32) * MOE_BLOCK
    block_e = jnp.minimum(jnp.searchsorted(pad_end, block_start, side='right'), N_EXPERTS - 1)

    def expert_block(args):
        e, tok_b, g_b = args
        xb = xf[tok_b]
        hb = jax.nn.silu(xb @ w_gate[e]) * (xb @ w_up[e])
        return (hb @ w_down[e]) * g_b[:, None].astype(xb.dtype)

    y = lax.map(expert_block, (block_e, row_tok.reshape(n_blocks, MOE_BLOCK), row_gate.reshape(n_blocks, MOE_BLOCK)))
    out = jnp.zeros_like(xf).at[row_tok].add(y.reshape(n_rows, d).astype(xf.dtype))
    return out.reshape(bsz, seq, d)


def modulate(x, c_act, w, b):
    m = (c_act @ w + b)[:, None, :]
    shift, scale, gate = jnp.split(m, 3, axis=-1)
    return x * (1.0 + scale) + shift, gate


def post_norm(x, gate, y, g, b):
    return layer_norm(ALPHA * x + (1.0 + gate) * y, g, b)


def setup_inputs(seed: int = 0) -> dict:
    key = jax.random.key(seed)
    ks = iter(jax.random.split(key, 32))

    def nrm(shape, std):
        return jax.random.normal(next(ks), shape, jnp.float32) * std

    D = D_MODEL
    ab_col_scale = jnp.concatenate([jnp.ones((A_Q_COLS + A_K_COLS,), jnp.float32),
                                    jnp.full((A_V_COLS,), BETA, jnp.float32),
                                    jnp.ones((B_Q_LORA + B_KV_LORA + B_ROPE,), jnp.float32)])
    ukv_col_scale = jnp.tile(jnp.concatenate([jnp.ones((B_NOPE,), jnp.float32),
                                              jnp.full((B_V_DIM,), BETA, jnp.float32)]), B_HEADS)
    c_col_scale = jnp.concatenate([jnp.ones((C_Q_COLS + C_KV_COLS,), jnp.float32),
                                   jnp.full((C_KV_COLS,), BETA, jnp.float32)])
    return {
        'x': nrm((BATCH, SEQ, D), 1.0),
        'c': nrm((BATCH, D), 1.0),
        'ada_w': nrm((DEPTH, 2, D, 3 * D), ADA_STD * D ** -0.5),
        'ada_b': nrm((DEPTH, 2, 3 * D), 0.02),
        'ln_g': 1.0 + nrm((DEPTH, 2, D), 0.02),
        'ln_b': nrm((DEPTH, 2, D), 0.02),
        'ab_w_in': nrm((N_EVEN, D, AB_IN_COLS), D ** -0.5) * ab_col_scale,
        'ab_lam': nrm((N_EVEN, 4, A_QK_DIM), 0.1),
        'ab_diff_g': 1.0 + nrm((N_EVEN, A_V_DIM), 0.02),
        'ab_q_norm_g': 1.0 + nrm((N_EVEN, B_Q_LORA), 0.02),
        'ab_kv_norm_g': 1.0 + nrm((N_EVEN, B_KV_LORA), 0.02),
        'ab_w_uq': nrm((N_EVEN, B_Q_LORA, B_HEADS * (B_NOPE + B_ROPE)), B_Q_LORA ** -0.5),
        'ab_w_ukv': nrm((N_EVEN, B_KV_LORA, B_HEADS * (B_NOPE + B_V_DIM)), B_KV_LORA ** -0.5) * ukv_col_scale,
        'ab_w_out': nrm((N_EVEN, AB_OUT_IN, D), BETA * AB_OUT_IN ** -0.5),
        'c_w_in': nrm((N_ODD, D, C_IN_COLS), D ** -0.5) * c_col_scale,
        'c_sink': nrm((N_ODD, C_HEADS), 0.5),
        'c_w_out': nrm((N_ODD, C_OUT_IN, D), BETA * C_OUT_IN ** -0.5),
        'ffn_w_gate': nrm((N_EVEN, D, D_FF), D ** -0.5),
        'ffn_w_up': nrm((N_EVEN, D, D_FF), D ** -0.5),
        'ffn_w_down': nrm((N_EVEN, D_FF, D), BETA * D_FF ** -0.5),
        'moe_w_router': nrm((N_ODD, D, N_EXPERTS), D ** -0.5),
        'moe_b_router': nrm((N_ODD, N_EXPERTS), 0.01),
        'moe_w_gate': nrm((N_ODD, N_EXPERTS, D, EXPERT_FF), D ** -0.5),
        'moe_w_up': nrm((N_ODD, N_EXPERTS, D, EXPERT_FF), D ** -0.5),
        'moe_w_down': nrm((N_ODD, N_EXPERTS, EXPERT_FF, D), BETA * EXPERT_FF ** -0.5),
    }


def reference(x, c, ada_w, ada_b, ln_g, ln_b, ab_w_in, ab_lam, ab_diff_g, ab_q_norm_g, ab_kv_norm_g,
              ab_w_uq, ab_w_ukv, ab_w_out, c_w_in, c_sink, c_w_out, ffn_w_gate, ffn_w_up, ffn_w_down,
              moe_w_router, moe_b_router, moe_w_gate, moe_w_up, moe_w_down):
    cos, sin = rope_tables(x.shape[1])
    c_act = jax.nn.silu(c)
    for layer in range(DEPTH):
        i = layer // 2
        h, gate = modulate(x, c_act, ada_w[layer, 0], ada_b[layer, 0])
        if layer % 2 == 0:
            lam_init = 0.8 - 0.6 * math.exp(-0.3 * layer)
            y = mixer_ab(h, ab_w_in[i], ab_lam[i], ab_diff_g[i], ab_q_norm_g[i], ab_kv_norm_g[i],
                         ab_w_uq[i], ab_w_ukv[i], ab_w_out[i], lam_init, cos, sin)
        else:
            y = mixer_c(h, c_w_in[i], c_sink[i], c_w_out[i])
        x = post_norm(x, gate, y, ln_g[layer, 0], ln_b[layer, 0])
        h, gate = modulate(x, c_act, ada_w[layer, 1], ada_b[layer, 1])
        if layer % 2 == 0:
            y = swiglu(h, ffn_w_gate[i], ffn_w_up[i], ffn_w_down[i])
        else:
            y = moe_swiglu(h, moe_w_router[i], moe_b_router[i], moe_w_gate[i], moe_w_up[i], moe_w_down[i])
        x = post_norm(x, gate, y, ln_g[layer, 1], ln_b[layer, 1])
    return x
```

```python
import contextlib
import math
import numpy as np
import concourse.bass as bass
import concourse.mybir as mybir
from concourse.bass_utils import run_bass_kernel_spmd

F32 = mybir.dt.float32
BF16 = mybir.dt.bfloat16
AF = mybir.ActivationFunctionType
ALU = mybir.AluOpType

D = 2048
KC = D // 128
DEPTH = 4
D_FF = 5632
N_EXP = 8
E_FF = 2816
ALPHA = (2 * DEPTH) ** 0.25
LN_EPS = 1e-5
RMS_EPS = 1e-6
NCORES = 8


class Prog:
    ENGS = ("pe", "act", "dve", "pool", "sp")

    def __init__(self, nc):
        self.nc = nc
        self.ops = {e: [] for e in self.ENGS}
        self.cnt = {}
        self.known = {e: {} for e in self.ENGS}
        self.last_w = {}
        self.readers = {}
        self.bar = {}

    def op(self, eng, meth, kw, reads=(), writes=(), dma=None):
        if dma is not None:
            sem, inc = "D_" + dma, 16
        else:
            sem, inc = "E_" + eng, 1
        deps = {}
        for s in tuple(reads) + tuple(writes):
            lw = self.last_w.get(s)
            if lw is not None and lw[1] > deps.get(lw[0], 0):
                deps[lw[0]] = lw[1]
        for s in writes:
            for sm, v in self.readers.get(s, ()):
                if v > deps.get(sm, 0):
                    deps[sm] = v
        for sm, v in self.bar.items():
            if v > deps.get(sm, 0):
                deps[sm] = v
        if dma is not None and self.cnt.get(sem, 0) > 0:
            deps[sem] = self.cnt[sem]
        waits = []
        kn = self.known[eng]
        for sm, v in deps.items():
            if eng == "pe" and sm == "E_pe":
                continue
            if kn.get(sm, 0) >= v:
                continue
            kn[sm] = v
            waits.append((sm, v))
        val = self.cnt.get(sem, 0) + inc
        self.cnt[sem] = val
        self.ops[eng].append((waits, (meth, kw), sem, inc))
        for s in writes:
            self.last_w[s] = (sem, val)
            self.readers[s] = []
        for s in reads:
            self.readers.setdefault(s, []).append((sem, val))
        return (sem, val)

    def barrier(self):
        self.bar = dict(self.cnt)

    def wait_all_dma(self, eng, classes):
        waits = [(n, v) for n, v in self.cnt.items() if n.startswith("D_")]
        self.ops[eng].append((waits, None, None, 0))

    def emit(self):
        nc = self.nc
        names = sorted(self.cnt.keys())
        with contextlib.ExitStack() as es:
            sems = {n: es.enter_context(nc.semaphore(n)) for n in names}
            block = es.enter_context(nc.Block())

            def replay(name):
                def body(e):
                    for waits, fn, sem, inc in self.ops[name]:
                        for sm, v in waits:
                            e.wait_ge(sems[sm], v)
                        if fn is not None:
                            getattr(e, fn[0])(*fn[1][0], **fn[1][1]).then_inc(sems[sem], inc)
                return body

            block.tensor(replay("pe"))
            block.scalar(replay("act"))
            block.vector(replay("dve"))
            block.gpsimd(replay("pool"))
            block.sync(replay("sp"))


def A(*a, **k):
    return (a, k)


class Ctx:
    pass


def _mm(P, out, lhsT, rhs, start, stop, reads, writes):
    P.op("pe", "matmul", A(out=out, lhsT=lhsT, rhs=rhs, start=start, stop=stop), reads=reads, writes=writes)


def phase_prologue(P, C, c_in, ada_w, ada_b, ln_g, ln_b, nsub_list):
    nc = P.nc
    es = C.es
    C.MOD = es.enter_context(nc.sbuf_tensor("MOD", [128, 8, 48], F32))
    C.LNG = es.enter_context(nc.sbuf_tensor("LNG", [128, 8, KC], F32))
    C.LNB = es.enter_context(nc.sbuf_tensor("LNB", [128, 8, KC], F32))
    C.ONESB = es.enter_context(nc.sbuf_tensor("ONESB", [128, 128], BF16))
    C.cact = es.enter_context(nc.sbuf_tensor("cact", [128, KC], F32))
    craw = es.enter_context(nc.sbuf_tensor("craw", [128, KC], F32))
    adab = es.enter_context(nc.sbuf_tensor("adab", [128, 8, 48], F32))
    P.op("pool", "memset", A(C.ONESB[:], 1.0 / D), writes=["ONESB"])
    P.op("sp", "dma_start", A(out=craw[:], in_=c_in.rearrange("(k p) -> p k", p=128),
                                     allow_slow_non_contiguous=True), writes=["craw"], dma="c")
    P.op("sp", "dma_start", A(out=C.LNG[:], in_=ln_g.rearrange("l s (k p) -> p (l s) k", p=128),
                                     allow_slow_non_contiguous=True), writes=["LNG"], dma="c")
    P.op("sp", "dma_start", A(out=C.LNB[:], in_=ln_b.rearrange("l s (k p) -> p (l s) k", p=128),
                                     allow_slow_non_contiguous=True), writes=["LNB"], dma="c")
    P.op("sp", "dma_start", A(out=adab[:], in_=ada_b.rearrange("l s (j p) -> p (l s) j", p=128),
                                     allow_slow_non_contiguous=True), writes=["adab"], dma="c")
    P.op("act", "activation", A(out=C.cact[:], in_=craw[:], func=AF.Silu),
         reads=["craw"], writes=["cact"])
    with contextlib.ExitStack() as ls:
        wbuf = [ls.enter_context(nc.sbuf_tensor(f"adaw{i}", [128, KC, 512], F32)) for i in range(2)]
        pm = ls.enter_context(nc.psum_tensor("pmod", [128, 512], F32))
        i = 0
        for s in nsub_list:
            layer, sub = divmod(s, 2)
            for q in range(12):
                wb = wbuf[i % 2]
                src = (ada_w[layer][sub] if isinstance(ada_w, dict) else ada_w[layer, sub])[:, q * 512:(q + 1) * 512].rearrange("(k p) f -> p k f", p=128)
                P.op("sp", "dma_start", A(out=wb[:], in_=src),
                     writes=[f"adaw{i % 2}"], dma=f"w{i % 2}")
                for jj in range(4):
                    j = q * 4 + jj
                    for k in range(KC):
                        first = (q == 0 and jj == 0 and k == 0)
                        _mm(P, pm[:, j:j + 1], wb[:, k, jj * 128:(jj + 1) * 128], C.cact[:, k:k + 1],
                            first, k == KC - 1, reads=[f"adaw{i % 2}", "cact"], writes=["pmod"])
                i += 1
            P.op("dve", "tensor_tensor", A(out=C.MOD[:, s, :], in0=pm[:, 0:48], in1=adab[:, s, :],
                                                       op=ALU.add),
                 reads=["pmod", "adab"], writes=["MOD"])
            P.op("dve", "tensor_scalar_add", A(out=C.MOD[:, s, 16:48], in0=C.MOD[:, s, 16:48],
                                                           scalar1=1.0),
                 reads=["MOD"], writes=["MOD"])


class Epi:
    def __init__(self, P, C, es, tag):
        nc = P.nc
        self.P, self.C, self.tag = P, C, tag
        self.xe = [es.enter_context(nc.sbuf_tensor(f"{tag}xe{i}", [128, 512], F32)) for i in range(2)]
        self.t = [es.enter_context(nc.sbuf_tensor(f"{tag}t{i}", [128, 512], F32)) for i in range(2)]
        self.zb = [es.enter_context(nc.sbuf_tensor(f"{tag}zb{i}", [128, 512], BF16)) for i in range(2)]
        self.zq = [es.enter_context(nc.sbuf_tensor(f"{tag}zq{i}", [128, 512], BF16)) for i in range(2)]
        self.mean = es.enter_context(nc.sbuf_tensor(f"{tag}mean", [128, 512], F32))
        self.rstd = es.enter_context(nc.sbuf_tensor(f"{tag}rstd", [128, 512], F32))
        self.nmr = es.enter_context(nc.sbuf_tensor(f"{tag}nmr", [128, 512], F32))
        self.xn = [es.enter_context(nc.sbuf_tensor(f"{tag}xn{i}", [128, 512], F32)) for i in range(2)]
        self.pmean_slot = f"{tag}pmean"
        self.pmsq_slot = f"{tag}pmsq"

    def load_x(self, f, xT_in, t0, in_slot):
        P, tag = self.P, self.tag
        xe = self.xe[f % 2]
        P.op("sp", "dma_start", A(out=xe[:], in_=xT_in[f * 128:(f + 1) * 128, t0:t0 + 512]),
             reads=[in_slot], writes=[f"{tag}xe{f % 2}"], dma=f"xe{f % 2}")

    def chunk(self, f, s, y_ap, y_slot, z, zslot, y_in_psum=True, zsuffix=""):
        P, C, tag = self.P, self.C, self.tag
        xe, t, zb, zq = self.xe[f % 2], self.t[f % 2], self.zb[f % 2], self.zq[f % 2]
        P.op("act", "activation", A(out=t[:], in_=y_ap, func=AF.Identity,
                                           scale=C.MOD[:, s, 32 + f:33 + f]),
             reads=[y_slot, "MOD"], writes=[f"{tag}t{f % 2}"])
        P.op("dve", "scalar_tensor_tensor", A(out=z[:, f, :], in0=xe[:], scalar=ALPHA, in1=t[:],
                                                     op0=ALU.mult, op1=ALU.add),
             reads=[f"{tag}xe{f % 2}", f"{tag}t{f % 2}"], writes=[f"{zslot}{f}{zsuffix}"])
        P.op("pool", "tensor_copy", A(out=zb[:], in_=z[:, f, :]),
             reads=[f"{zslot}{f}{zsuffix}"], writes=[f"{tag}zb{f % 2}"])
        P.op("act", "activation", A(out=zq[:], in_=z[:, f, :], func=AF.Square),
             reads=[f"{zslot}{f}{zsuffix}"], writes=[f"{tag}zq{f % 2}"])

    def stats_mm(self, f, pmean, pmsq):
        P, C, tag = self.P, self.C, self.tag
        zb, zq = self.zb[f % 2], self.zq[f % 2]
        _mm(P, pmean[:], C.ONESB[:], zb[:], f == 0, f == KC - 1,
            reads=[f"{tag}zb{f % 2}", "ONESB"], writes=[self.pmean_slot])
        _mm(P, pmsq[:], C.ONESB[:], zq[:], f == 0, f == KC - 1,
            reads=[f"{tag}zq{f % 2}", "ONESB"], writes=[self.pmsq_slot])

    def finish(self, s, pmean, pmsq, z, zslot, xT_out, t0, out_slot, zsuffix=""):
        P, C, tag = self.P, self.C, self.tag
        mean, rstd, nmr = self.mean, self.rstd, self.nmr
        P.op("act", "activation", A(out=mean[:], in_=pmean[:], func=AF.Identity),
             reads=[self.pmean_slot], writes=[f"{tag}mean"])
        P.op("dve", "tensor_tensor", A(out=nmr[:], in0=mean[:], in1=mean[:], op=ALU.mult),
             reads=[f"{tag}mean"], writes=[f"{tag}nmr"])
        P.op("dve", "tensor_tensor", A(out=rstd[:], in0=pmsq[:], in1=nmr[:], op=ALU.subtract),
             reads=[self.pmsq_slot, f"{tag}nmr"], writes=[f"{tag}rstd"])
        P.op("dve", "tensor_scalar_add", A(out=rstd[:], in0=rstd[:], scalar1=LN_EPS),
             reads=[f"{tag}rstd"], writes=[f"{tag}rstd"])
        P.op("act", "activation", A(out=rstd[:], in_=rstd[:], func=AF.Sqrt),
             reads=[f"{tag}rstd"], writes=[f"{tag}rstd"])
        P.op("dve", "reciprocal", A(out=rstd[:], in_=rstd[:]),
             reads=[f"{tag}rstd"], writes=[f"{tag}rstd"])
        P.op("dve", "scalar_tensor_tensor", A(out=nmr[:], in0=mean[:], scalar=-1.0, in1=rstd[:],
                                                     op0=ALU.mult, op1=ALU.mult),
             reads=[f"{tag}mean", f"{tag}rstd"], writes=[f"{tag}nmr"])
        for f in range(KC):
            xn = self.xn[f % 2]
            P.op("dve", "tensor_tensor", A(out=xn[:], in0=z[:, f, :], in1=rstd[:], op=ALU.mult),
                 reads=[f"{zslot}{f}{zsuffix}", f"{tag}rstd"], writes=[f"{tag}xn{f % 2}"])
            P.op("pool", "tensor_tensor", A(out=xn[:], in0=xn[:], in1=nmr[:], op=ALU.add),
                 reads=[f"{tag}xn{f % 2}", f"{tag}nmr"], writes=[f"{tag}xn{f % 2}"])
            P.op("act", "activation", A(out=xn[:], in_=xn[:], func=AF.Identity,
                                        scale=C.LNG[:, s, f:f + 1], bias=C.LNB[:, s, f:f + 1]),
                 reads=[f"{tag}xn{f % 2}", "LNG", "LNB"], writes=[f"{tag}xn{f % 2}"])
            P.op("sp", "dma_start", A(out=xT_out[f * 128:(f + 1) * 128, t0:t0 + 512], in_=xn[:]),
                 reads=[f"{tag}xn{f % 2}"], writes=[out_slot], dma=f"xo{f % 2}")


def phase_ffn(P, C, xT_in, in_slot, xT_out, out_slot, wg, wu, wd, s, T, TB=1024):
    nc = P.nc
    P.barrier()
    NJ = TB // 512
    NFF = D_FF // 128
    with contextlib.ExitStack() as es:
        hz = es.enter_context(nc.sbuf_tensor("f_hz", [128, KC * 512], F32))
        zt = hz[:].rearrange("p (k t) -> p k t", k=KC)
        hTt = hz[:].bitcast(BF16).rearrange("p (k t) -> p k t", k=KC)
        act = es.enter_context(nc.sbuf_tensor("f_act", [128, NFF, TB], BF16))
        wgu = [es.enter_context(nc.sbuf_tensor(f"f_wgu{i}", [128, 2, KC, 128], BF16)) for i in range(2)]
        wdb = [es.enter_context(nc.sbuf_tensor(f"f_wd{i}", [128, NFF, 128], BF16)) for i in range(2)]
        xs = [es.enter_context(nc.sbuf_tensor(f"f_xs{i}", [128, 512], F32)) for i in range(2)]
        sg = [es.enter_context(nc.sbuf_tensor(f"f_sg{i}", [128, 512], F32)) for i in range(2)]
        pg = [es.enter_context(nc.psum_tensor(f"f_pg{i}", [128, 512], F32)) for i in range(2)]
        pu = [es.enter_context(nc.psum_tensor(f"f_pu{i}", [128, 512], F32)) for i in range(2)]
        py = [es.enter_context(nc.psum_tensor(f"f_py{i}", [128, 512], F32)) for i in range(2)]
        pmean = es.enter_context(nc.psum_tensor("f_pmean", [128, 512], F32))
        pmsq = es.enter_context(nc.psum_tensor("f_pmsq", [128, 512], F32))
        epi = Epi(P, C, es, "f_")
        wcnt = 0
        dcnt = 0
        for tb in range(T // TB):
            tbase = tb * TB
            for k in range(KC):
                for j in range(NJ):
                    b = xs[(k * NJ + j) % 2]
                    bs = f"f_xs{(k * NJ + j) % 2}"
                    P.op("sp", "dma_start", A(
                        out=b[:], in_=xT_in[k * 128:(k + 1) * 128, tbase + j * 512:tbase + (j + 1) * 512]),
                        reads=[in_slot], writes=[bs], dma=f"xs{(k * NJ + j) % 2}")
                    P.op("act", "activation", A(
                        out=hTt[:, k, j * 512:(j + 1) * 512], in_=b[:], func=AF.Identity,
                        scale=C.MOD[:, s, 16 + k:17 + k], bias=C.MOD[:, s, k:k + 1]),
                        reads=[bs, "MOD"], writes=[f"f_hz{k}"])
            hslots = [f"f_hz{k}" for k in range(KC)]
            for c in range(NFF):
                wb = wgu[wcnt % 2]
                wslot = f"f_wgu{wcnt % 2}"
                wcnt += 1
                P.op("pool", "dma_start", A(
                    out=wb[:, 0], in_=wg[:, c * 128:(c + 1) * 128].rearrange("(k p) f -> p k f", p=128)),
                    writes=[wslot + "g"], dma=f"w{(wcnt - 1) % 2}")
                P.op("pool", "dma_start", A(
                    out=wb[:, 1], in_=wu[:, c * 128:(c + 1) * 128].rearrange("(k p) f -> p k f", p=128)),
                    writes=[wslot + "u"], dma=f"w{2 + (wcnt - 1) % 2}")
                for j in range(NJ):
                    par = (c * NJ + j) % 2
                    for k in range(KC):
                        _mm(P, pg[par][:], wb[:, 0, k, :], hTt[:, k, j * 512:(j + 1) * 512], k == 0, k == KC - 1,
                            reads=[wslot + "g", hslots[k]], writes=[f"f_pg{par}"])
                    for k in range(KC):
                        _mm(P, pu[par][:], wb[:, 1, k, :], hTt[:, k, j * 512:(j + 1) * 512], k == 0, k == KC - 1,
                            reads=[wslot + "u", hslots[k]], writes=[f"f_pu{par}"])
                    P.op("act", "activation", A(out=sg[par][:], in_=pg[par][:], func=AF.Silu),
                         reads=[f"f_pg{par}"], writes=[f"f_sg{par}"])
                    P.op("dve", "tensor_tensor", A(
                        out=act[:, c, j * 512:(j + 1) * 512], in0=sg[par][:], in1=pu[par][:], op=ALU.mult),
                        reads=[f"f_sg{par}", f"f_pu{par}"], writes=[f"f_act{c}_{j}"])
            for j in range(NJ):
                t0 = tbase + j * 512
                pend = None
                for f in range(KC):
                    wd_t = wdb[dcnt % 2]
                    dslot = f"f_wd{dcnt % 2}"
                    dcnt += 1
                    P.op("pool", "dma_start", A(
                        out=wd_t[:], in_=wd[:, f * 128:(f + 1) * 128].rearrange("(c p) f -> p c f", p=128)),
                        writes=[dslot], dma=f"wd{(dcnt - 1) % 2}")
                    epi.load_x(f, xT_in, t0, in_slot)
                    for c in range(NFF):
                        _mm(P, py[f % 2][:], wd_t[:, c, :], act[:, c, j * 512:(j + 1) * 512], c == 0, c == NFF - 1,
                            reads=[dslot, f"f_act{c}_{j}"], writes=[f"f_py{f % 2}"])
                    epi.chunk(f, s, py[f % 2][:], f"f_py{f % 2}", zt, "f_hz")
                    if pend is not None:
                        epi.stats_mm(pend, pmean, pmsq)
                    pend = f
                epi.stats_mm(pend, pmean, pmsq)
                epi.finish(s, pmean, pmsq, zt, "f_hz", xT_out, t0, out_slot)


def load_hT(P, C, xT_in, in_slot, hTt, xs, s, tbase, TB, tag):
    NJ = TB // 512
    for k in range(KC):
        for j in range(NJ):
            b = xs[(k * NJ + j) % 2]
            bs = f"{tag}xs{(k * NJ + j) % 2}"
            P.op("sp", "dma_start", A(
                out=b[:], in_=xT_in[k * 128:(k + 1) * 128, tbase + j * 512:tbase + (j + 1) * 512]),
                reads=[in_slot], writes=[bs], dma=f"xs{(k * NJ + j) % 2}")
            P.op("act", "activation", A(
                out=hTt[:, k, j * 512:(j + 1) * 512], in_=b[:], func=AF.Identity,
                scale=C.MOD[:, s, 16 + k:17 + k], bias=C.MOD[:, s, k:k + 1]),
                reads=[bs, "MOD"], writes=[f"{tag}hT{k}"])
    return [f"{tag}hT{k}" for k in range(KC)]


def proj_fm(P, w_ap, col0, ncols, hTt, hslots, j, wb, wslot, ps, pslot):
    for k in range(KC):
        _mm(P, ps[0:ncols, :], wb[:, k, 0:ncols], hTt[:, k, j * 512:(j + 1) * 512], k == 0, k == KC - 1,
            reads=[wslot, hslots[k]], writes=[pslot])


def phase_c_proj(P, C, xT_in, in_slot, w_in, QT, KT, V, s, T, TB=1024):
    nc = P.nc
    P.barrier()
    NJ = TB // 512
    with contextlib.ExitStack() as es:
        hTt = es.enter_context(nc.sbuf_tensor("cp_hT", [128, KC, TB], BF16))
        xs = [es.enter_context(nc.sbuf_tensor(f"cp_xs{i}", [128, 512], F32)) for i in range(2)]
        wb = [es.enter_context(nc.sbuf_tensor(f"cp_w{i}", [128, KC, 128], BF16)) for i in range(2)]
        wv = es.enter_context(nc.sbuf_tensor("cp_wv", [128, KC, 512], BF16))
        st = [es.enter_context(nc.sbuf_tensor(f"cp_st{i}", [128, 512], BF16)) for i in range(2)]
        ps = [es.enter_context(nc.psum_tensor(f"cp_ps{i}", [128, 512], F32)) for i in range(2)]
        for q4 in range(4):
            P.op("pool", "dma_start", A(out=wv[:, :, q4 * 128:(q4 + 1) * 128],
                                        in_=w_in[:, 2560 + q4 * 128:2688 + q4 * 128].rearrange("(k p) f -> p k f", p=128)),
                 writes=["cp_wv"], dma="w2")
        wcnt = 0
        n = 0
        for tb in range(T // TB):
            tbase = tb * TB
            hslots = load_hT(P, C, xT_in, in_slot, hTt, xs, s, tbase, TB, "cp_")
            for c in range(20):
                w = wb[wcnt % 2]
                wslot = f"cp_w{wcnt % 2}"
                wcnt += 1
                P.op("pool", "dma_start", A(
                    out=w[:], in_=w_in[:, c * 128:(c + 1) * 128].rearrange("(k p) f -> p k f", p=128)),
                    writes=[wslot], dma=f"w{(wcnt - 1) % 2}")
                for j in range(NJ):
                    par = n % 2
                    n += 1
                    proj_fm(P, w_in, 0, 128, hTt, hslots, j, w, wslot, ps[par], f"cp_ps{par}")
                    sc = (128 ** -0.5) if c < 16 else 1.0
                    P.op("act", "activation", A(out=st[par][:], in_=ps[par][:],
                                                                       func=AF.Copy, scale=sc),
                         reads=[f"cp_ps{par}"], writes=[f"cp_st{par}"])
                    dst = QT if c < 16 else KT
                    r0 = (c if c < 16 else c - 16) * 128
                    P.op("sp", "dma_start", A(
                        out=dst[r0:r0 + 128, tbase + j * 512:tbase + (j + 1) * 512], in_=st[par][:]),
                        reads=[f"cp_st{par}"], writes=["c_qkv"], dma=f"st{par}")
            for tt in range(TB // 128):
                par = n % 2
                n += 1
                for k in range(KC):
                    _mm(P, ps[par][:], hTt[:, k, tt * 128:(tt + 1) * 128], wv[:, k, :], k == 0, k == KC - 1,
                        reads=["cp_wv", hslots[k]], writes=[f"cp_ps{par}"])
                P.op("dve", "tensor_copy", A(out=st[par][:], in_=ps[par][:]),
                     reads=[f"cp_ps{par}"], writes=[f"cp_st{par}"])
                P.op("sp", "dma_start", A(
                    out=V[tbase + tt * 128:tbase + (tt + 1) * 128, :], in_=st[par][:]),
                    reads=[f"cp_st{par}"], writes=["c_qkv"], dma=f"st{par}")


def phase_c_attn(P, C, xT_in, in_slot, xT_out, out_slot, QT, KT, V, KTh, Vh, HB, CT, sink, w_out, s, T):
    nc = P.nc
    P.barrier()
    NB = T // 128
    slopes = [2.0 ** (-8.0 * (h + 1) / 16) for h in range(16)]
    with contextlib.ExitStack() as es:
        kt = es.enter_context(nc.sbuf_tensor("ca_kt", [128, 4, T + 256], BF16))
        v = es.enter_context(nc.sbuf_tensor("ca_v", [128, NB + 2, 512], BF16))
        qt = es.enter_context(nc.sbuf_tensor("ca_qt", [128, 16, 512], BF16))
        ot = es.enter_context(nc.sbuf_tensor("ca_ot", [128, 16, 512], BF16))
        ndist = es.enter_context(nc.sbuf_tensor("ca_nd", [128, 384], F32))
        hb = es.enter_context(nc.sbuf_tensor("ca_hb", [128, 2], F32))
        esb = es.enter_context(nc.sbuf_tensor("ca_es", [128, 16], F32))
        esbc = es.enter_context(nc.sbuf_tensor("ca_esbc", [128, 16, 128], F32))
        onesk = es.enter_context(nc.sbuf_tensor("ca_ones", [128, 128], BF16))
        tmp = [es.enter_context(nc.sbuf_tensor(f"ca_tmp{i}", [128, 512], F32)) for i in range(3)]
        pT = [es.enter_context(nc.sbuf_tensor(f"ca_pT{i}", [128, 512], BF16)) for i in range(3)]
        den = es.enter_context(nc.sbuf_tensor("ca_den", [128, 512], F32))
        wo = [es.enter_context(nc.sbuf_tensor(f"ca_wo{i}", [128, 16, 128], BF16)) for i in range(2)]
        zt = es.enter_context(nc.sbuf_tensor("ca_z", [128, KC, 512], F32))
        psS = [es.enter_context(nc.psum_tensor(f"ca_ps{i}", [128, 512], F32)) for i in range(3)]
        pO = es.enter_context(nc.psum_tensor("ca_pO", [128, 512], F32))
        pL = es.enter_context(nc.psum_tensor("ca_pL", [128, 512], F32))
        py = es.enter_context(nc.psum_tensor("ca_py", [128, 512], F32))
        pmean = es.enter_context(nc.psum_tensor("ca_pmean", [128, 512], F32))
        pmsq = es.enter_context(nc.psum_tensor("ca_pmsq", [128, 512], F32))
        epi = Epi(P, C, es, "ca_")
        P.op("sp", "dma_start", A(out=ndist[:], in_=CT[:, 0:384]), writes=["ca_nd"], dma="c")
        P.op("sp", "dma_start", A(out=hb[:], in_=HB), writes=["ca_hb"], dma="c")
        P.op("sp", "dma_start", A(out=esb[:], in_=sink.partition_broadcast(128)), writes=["ca_es"], dma="c")
        P.op("act", "activation", A(out=esb[:], in_=esb[:], func=AF.Exp), reads=["ca_es"], writes=["ca_es"])
        P.op("pool", "memset", A(onesk[:], 1.0), writes=["ca_ones"])
        for h in range(16):
            P.op("dve", "tensor_copy", A(out=esbc[:, h, :], in_=esb[:, h:h + 1].to_broadcast([128, 128])),
                 reads=["ca_es"], writes=["ca_esbc"])
        for n in range(4):
            P.op("sp", "dma_start", A(out=kt[:, n, 0:T], in_=KT[n * 128:(n + 1) * 128, :]),
                 reads=["c_qkv"], writes=["ca_kt"], dma=f"kv{n}")
            P.op("sp", "dma_start", A(out=kt[:, n, T:T + 256].rearrange("p (i t) -> p i t", i=2),
                                                   in_=KTh[:, n * 128:(n + 1) * 128, :].rearrange("i p t -> p i t")),
                 reads=["c_halo"], writes=["ca_kt"], dma=f"kv{n}")
        P.op("sp", "dma_start", A(out=v[:, 0:NB, :], in_=V.rearrange("(b p) f -> p b f", p=128)),
             reads=["c_qkv"], writes=["ca_v"], dma="kv4")
        P.op("sp", "dma_start", A(out=v[:, NB:NB + 2, :], in_=Vh.rearrange("i p f -> p i f")),
             reads=["c_halo"], writes=["ca_v"], dma="kv5")
        wcnt = 0
        si = 0
        for grp in range(T // 512):
            t0 = grp * 512
            for hq in range(4):
                P.op("sp", "dma_start", A(
                    out=qt[:, hq * 4:(hq + 1) * 4, :],
                    in_=QT[hq * 512:(hq + 1) * 512, t0:t0 + 512].rearrange("(h p) t -> p h t", p=128)),
                    reads=["c_qkv"], writes=[f"ca_qt{hq}"], dma=f"kv{hq}")
            for ql in range(4):
                qb = grp * 4 + ql
                for n in range(4):
                    kbs = []
                    for typ, kb in ((0, qb - 1), (1, qb), (2, qb + 1)):
                        if kb < 0:
                            kbs.append((typ, NB, 0))
                        elif kb >= NB:
                            kbs.append((typ, NB + 1, 1))
                        else:
                            kbs.append((typ, kb, None))
                    used = []
                    for typ, kb, halo in kbs:
                        i3 = si % 3
                        si += 1
                        used.append(i3)
                        _mm(P, psS[i3][:], kt[:, n, kb * 128:(kb + 1) * 128], qt[:, n * 4:(n + 1) * 4, ql * 128:(ql + 1) * 128],
                            True, True, reads=["ca_kt", f"ca_qt{n}"], writes=[f"ca_ps{i3}"])
                        for g in range(4):
                            h = n * 4 + g
                            P.op("dve", "scalar_tensor_tensor", A(
                                out=tmp[i3][:, g * 128:(g + 1) * 128], in0=ndist[:, typ * 128:(typ + 1) * 128],
                                scalar=slopes[h], in1=psS[i3][:, g * 128:(g + 1) * 128], op0=ALU.mult, op1=ALU.add),
                                reads=[f"ca_ps{i3}", "ca_nd"], writes=[f"ca_tmp{i3}"])
                        if halo is None:
                            P.op("act", "activation", A(out=pT[i3][:], in_=tmp[i3][:], func=AF.Exp),
                                 reads=[f"ca_tmp{i3}"], writes=[f"ca_pT{i3}"])
                        else:
                            P.op("act", "activation", A(
                                out=pT[i3][:], in_=tmp[i3][:], func=AF.Exp, bias=hb[:, halo:halo + 1]),
                                reads=[f"ca_tmp{i3}", "ca_hb"], writes=[f"ca_pT{i3}"])
                    for ii, ((typ, kb, halo), i3) in enumerate(zip(kbs, used)):
                        _mm(P, pO[:], v[:, kb, n * 128:(n + 1) * 128], pT[i3][:], ii == 0, ii == 2,
                            reads=["ca_v", f"ca_pT{i3}"], writes=["ca_pO"])
                    for ii, ((typ, kb, halo), i3) in enumerate(zip(kbs, used)):
                        _mm(P, pL[:], onesk[:], pT[i3][:], ii == 0, ii == 2,
                            reads=["ca_ones", f"ca_pT{i3}"], writes=["ca_pL"])
                    P.op("dve", "tensor_tensor", A(
                        out=den[:], in0=pL[:], in1=esbc[:, n * 4:(n + 1) * 4, :].rearrange("p h t -> p (h t)"), op=ALU.add),
                        reads=["ca_pL", "ca_esbc"], writes=["ca_den"])
                    P.op("dve", "reciprocal", A(out=den[:], in_=den[:]), reads=["ca_den"], writes=["ca_den"])
                    P.op("dve", "tensor_tensor", A(
                        out=ot[:, n * 4:(n + 1) * 4, ql * 128:(ql + 1) * 128],
                        in0=pO[:].rearrange("p (h t) -> p h t", h=4), in1=den[:].rearrange("p (h t) -> p h t", h=4),
                        op=ALU.mult),
                        reads=["ca_pO", "ca_den"], writes=[f"ca_ot{n}"])
            pend = None
            for f in range(KC):
                w = wo[wcnt % 2]
                wslot = f"ca_wo{wcnt % 2}"
                wcnt += 1
                P.op("pool", "dma_start", A(
                    out=w[:], in_=w_out[:, f * 128:(f + 1) * 128].rearrange("(h p) f -> p h f", p=128)),
                    writes=[wslot], dma=f"w{(wcnt - 1) % 2}")
                epi.load_x(f, xT_in, t0, in_slot)
                for h in range(16):
                    _mm(P, py[:], w[:, h, :], ot[:, h, :], h == 0, h == 15,
                        reads=[wslot, f"ca_ot{h // 4}"], writes=["ca_py"])
                epi.chunk(f, s, py[:], "ca_py", zt, "ca_z")
                if pend is not None:
                    epi.stats_mm(pend, pmean, pmsq)
                pend = f
            epi.stats_mm(pend, pmean, pmsq)
            epi.finish(s, pmean, pmsq, zt, "ca_z", xT_out, t0, out_slot)


def c_tables():
    ik = np.arange(128)[:, None]
    jq = np.arange(128)[None, :]
    out = np.zeros((128, 384), np.float32)
    for typ, off in ((0, -128), (1, 0), (2, 128)):
        dist = np.abs((off + ik) - jq).astype(np.float32)
        out[:, typ * 128:(typ + 1) * 128] = np.where(dist <= 128, -dist, -1e6)
    return out


def phase_moe(P, C, xT_in, in_slot, xT_out, out_slot, w_router, b_router, wg, wu, wd, s, T, TB=1024):
    nc = P.nc
    P.barrier()
    NJ = TB // 512
    NT = TB // 128
    NFF = E_FF // 128
    with contextlib.ExitStack() as es:
        hTt = es.enter_context(nc.sbuf_tensor("m_hT", [128, KC, TB], BF16))
        yacc = es.enter_context(nc.sbuf_tensor("m_yacc", [128, KC, TB], F32))
        act = es.enter_context(nc.sbuf_tensor("m_act", [128, NFF, TB], BF16))
        wgu = [es.enter_context(nc.sbuf_tensor(f"m_wgu{i}", [128, 2, KC, 128], BF16)) for i in range(2)]
        wdb = [es.enter_context(nc.sbuf_tensor(f"m_wd{i}", [128, NFF, 128], BF16)) for i in range(2)]
        sg = [es.enter_context(nc.sbuf_tensor(f"m_sg{i}", [128, 512], F32)) for i in range(2)]
        gb = [es.enter_context(nc.sbuf_tensor("m_gb0", [128, TB], F32))] * 2
        wr = es.enter_context(nc.sbuf_tensor("m_wr", [128, KC, N_EXP], F32))
        br = es.enter_context(nc.sbuf_tensor("m_br", [128, N_EXP], F32))
        ident = es.enter_context(nc.sbuf_tensor("m_ident", [128, 128], F32))
        G = es.enter_context(nc.sbuf_tensor("m_G", [128, NT, N_EXP], F32))
        Gb = [es.enter_context(nc.sbuf_tensor(f"m_Gb{i}", [128, 128], F32)) for i in range(2)]
        sm = es.enter_context(nc.sbuf_tensor("m_sm", [128, 64], F32))
        lg = es.enter_context(nc.sbuf_tensor("m_lg", [128, N_EXP], F32))
        lg2 = es.enter_context(nc.sbuf_tensor("m_lg2", [128, N_EXP], F32))
        eq1 = es.enter_context(nc.sbuf_tensor("m_eq1", [128, N_EXP], F32))
        eq2 = es.enter_context(nc.sbuf_tensor("m_eq2", [128, N_EXP], F32))
        pg = [es.enter_context(nc.psum_tensor(f"m_pg{i}", [128, 512], F32)) for i in range(2)]
        pu = [es.enter_context(nc.psum_tensor(f"m_pu{i}", [128, 512], F32)) for i in range(2)]
        py = [es.enter_context(nc.psum_tensor(f"m_py{i}", [128, 512], F32)) for i in range(2)]
        plg = es.enter_context(nc.psum_tensor("m_plg", [128, 512], F32))
        pst = es.enter_context(nc.psum_tensor("m_pst", [128, 512], F32))
        pst2 = pg[0]
        epi = Epi(P, C, es, "m_")
        epi.pmean_slot, epi.pmsq_slot = "m_pst", "m_pg0"
        xs, h32 = epi.xe, epi.t
        P.op("sp", "dma_start", A(out=wr[:], in_=w_router.rearrange("(k p) e -> p k e", p=128)), writes=["m_wr"], dma="c")
        P.op("sp", "dma_start", A(out=br[:], in_=b_router.partition_broadcast(128)), writes=["m_br"], dma="c")
        P.op("sp", "dma_start", A(out=ident[:], in_=C.ident_dram), writes=["m_ident"], dma="c")
        wcnt = 0
        dcnt = 0
        n = 0
        for tb in range(T // TB):
            tbase = tb * TB
            for k in range(KC):
                for j in range(NJ):
                    i2 = (k * NJ + j) % 2
                    P.op("sp", "dma_start", A(out=xs[i2][:], in_=xT_in[k * 128:(k + 1) * 128, tbase + j * 512:tbase + (j + 1) * 512]),
                         reads=[in_slot], writes=[f"m_xe{i2}"], dma=f"xe{i2}")
                    P.op("act", "activation", A(out=h32[i2][:], in_=xs[i2][:], func=AF.Identity,
                                                scale=C.MOD[:, s, 16 + k:17 + k], bias=C.MOD[:, s, k:k + 1]),
                         reads=[f"m_xe{i2}", "MOD"], writes=[f"m_t{i2}"])
                    P.op("pool", "tensor_copy", A(out=hTt[:, k, j * 512:(j + 1) * 512], in_=h32[i2][:]),
                         reads=[f"m_t{i2}"], writes=[f"m_hT{k}"])
                    for t4 in range(4):
                        tt = j * 4 + t4
                        first = (k == 0 and j == 0 and t4 == 0)
                        _mm(P, plg[:, tt * 8:(tt + 1) * 8], h32[i2][:, t4 * 128:(t4 + 1) * 128], wr[:, k, :], first, k == KC - 1,
                            reads=[f"m_t{i2}", "m_wr"], writes=["m_plg"])
            hslots = [f"m_hT{k}" for k in range(KC)]
            for tt in range(NT):
                P.op("dve", "tensor_tensor", A(out=lg[:], in0=plg[:, tt * 8:(tt + 1) * 8], in1=br[:], op=ALU.add),
                     reads=["m_plg", "m_br"], writes=["m_lg"])
                P.op("dve", "tensor_reduce", A(out=sm[:, 0:1], in_=lg[:], axis=mybir.AxisListType.X, op=ALU.max),
                     reads=["m_lg"], writes=["m_sm0"])
                P.op("dve", "tensor_scalar", A(out=eq1[:], in0=lg[:], scalar1=sm[:, 0:1], scalar2=None, op0=ALU.is_equal),
                     reads=["m_lg", "m_sm0"], writes=["m_eq1"])
                P.op("dve", "scalar_tensor_tensor", A(out=lg2[:], in0=eq1[:], scalar=-1e30, in1=lg[:], op0=ALU.mult, op1=ALU.add),
                     reads=["m_eq1", "m_lg"], writes=["m_lg2"])
                P.op("dve", "tensor_reduce", A(out=sm[:, 1:2], in_=lg2[:], axis=mybir.AxisListType.X, op=ALU.max),
                     reads=["m_lg2"], writes=["m_sm1"])
                P.op("dve", "tensor_scalar", A(out=eq2[:], in0=lg2[:], scalar1=sm[:, 1:2], scalar2=None, op0=ALU.is_equal),
                     reads=["m_lg2", "m_sm1"], writes=["m_eq2"])
                P.op("dve", "tensor_tensor", A(out=sm[:, 2:3], in0=sm[:, 1:2], in1=sm[:, 0:1], op=ALU.subtract),
                     reads=["m_sm0", "m_sm1"], writes=["m_sm2"])
                P.op("act", "activation", A(out=sm[:, 3:4], in_=sm[:, 2:3], func=AF.Exp), reads=["m_sm2"], writes=["m_sm3"])
                P.op("dve", "tensor_scalar_add", A(out=sm[:, 4:5], in0=sm[:, 3:4], scalar1=1.0), reads=["m_sm3"], writes=["m_sm4"])
                P.op("dve", "reciprocal", A(out=sm[:, 5:6], in_=sm[:, 4:5]), reads=["m_sm4"], writes=["m_sm5"])
                P.op("dve", "tensor_tensor", A(out=sm[:, 6:7], in0=sm[:, 3:4], in1=sm[:, 5:6], op=ALU.mult),
                     reads=["m_sm3", "m_sm5"], writes=["m_sm6"])
                P.op("dve", "tensor_scalar", A(out=eq1[:], in0=eq1[:], scalar1=sm[:, 5:6], scalar2=None, op0=ALU.mult),
                     reads=["m_eq1", "m_sm5"], writes=["m_eq1"])
                P.op("dve", "scalar_tensor_tensor", A(out=G[:, tt, :], in0=eq2[:], scalar=sm[:, 6:7], in1=eq1[:],
                                                      op0=ALU.mult, op1=ALU.add),
                     reads=["m_eq2", "m_sm6", "m_eq1"], writes=[f"m_G{tt}"])
            for ex in range(N_EXP):
                gbe = gb[ex % 2]
                gslot = "m_gb0"
                for tt in range(NT):
                    g2 = Gb[tt % 2]
                    P.op("dve", "tensor_copy", A(out=g2[:], in_=G[:, tt, ex:ex + 1].to_broadcast([128, 128])),
                         reads=[f"m_G{tt}"], writes=[f"m_Gb{tt % 2}"])
                    _mm(P, plg[:, (tt % 4) * 128:(tt % 4 + 1) * 128], g2[:], ident[:], tt % 4 == 0, True,
                        reads=[f"m_Gb{tt % 2}", "m_ident"], writes=["m_plg"])
                    if tt % 4 == 3:
                        P.op("act", "activation", A(out=gbe[:, (tt // 4) * 512:(tt // 4 + 1) * 512], in_=plg[:], func=AF.Identity),
                             reads=["m_plg"], writes=[gslot])
                for c in range(NFF):
                    wb = wgu[wcnt % 2]
                    wslot = f"m_wgu{wcnt % 2}"
                    wi = wcnt % 2
                    wcnt += 1
                    P.op("pool", "dma_start", A(out=wb[:, 0], in_=wg[ex, :, c * 128:(c + 1) * 128].rearrange("(k p) f -> p k f", p=128)),
                         writes=[wslot + "g"], dma=f"w{wi}")
                    P.op("pool", "dma_start", A(out=wb[:, 1], in_=wu[ex, :, c * 128:(c + 1) * 128].rearrange("(k p) f -> p k f", p=128)),
                         writes=[wslot + "u"], dma=f"w{2 + wi}")
                    for j in range(NJ):
                        par = n % 2
                        n += 1
                        for k in range(KC):
                            _mm(P, pg[par][:], wb[:, 0, k, :], hTt[:, k, j * 512:(j + 1) * 512], k == 0, k == KC - 1,
                                reads=[wslot + "g", hslots[k]], writes=[f"m_pg{par}"])
                        for k in range(KC):
                            _mm(P, pu[par][:], wb[:, 1, k, :], hTt[:, k, j * 512:(j + 1) * 512], k == 0, k == KC - 1,
                                reads=[wslot + "u", hslots[k]], writes=[f"m_pu{par}"])
                        P.op("act", "activation", A(out=sg[par][:], in_=pg[par][:], func=AF.Silu),
                             reads=[f"m_pg{par}"], writes=[f"m_sg{par}"])
                        P.op("pool", "tensor_tensor", A(out=sg[par][:], in0=sg[par][:], in1=gbe[:, j * 512:(j + 1) * 512], op=ALU.mult),
                             reads=[f"m_sg{par}", gslot], writes=[f"m_sg{par}"])
                        P.op("dve", "tensor_tensor", A(out=act[:, c, j * 512:(j + 1) * 512], in0=sg[par][:], in1=pu[par][:], op=ALU.mult),
                             reads=[f"m_sg{par}", f"m_pu{par}"], writes=[f"m_act{c}_{j}"])
                for f in range(KC):
                    wd_t = wdb[dcnt % 2]
                    dslot = f"m_wd{dcnt % 2}"
                    di = dcnt % 2
                    dcnt += 1
                    P.op("pool", "dma_start", A(out=wd_t[:], in_=wd[ex, :, f * 128:(f + 1) * 128].rearrange("(c p) f -> p c f", p=128)),
                         writes=[dslot], dma=f"wd{di}")
                    for j in range(NJ):
                        par = n % 2
                        n += 1
                        for c in range(NFF):
                            _mm(P, py[par][:], wd_t[:, c, :], act[:, c, j * 512:(j + 1) * 512], c == 0, c == NFF - 1,
                                reads=[dslot, f"m_act{c}_{j}"], writes=[f"m_py{par}"])
                        if ex == 0:
                            P.op("act", "activation", A(out=yacc[:, f, j * 512:(j + 1) * 512], in_=py[par][:], func=AF.Identity),
                                 reads=[f"m_py{par}"], writes=[f"m_y{f}_{j}"])
                        else:
                            P.op("dve", "tensor_tensor", A(out=yacc[:, f, j * 512:(j + 1) * 512], in0=py[par][:],
                                                           in1=yacc[:, f, j * 512:(j + 1) * 512], op=ALU.add),
                                 reads=[f"m_py{par}", f"m_y{f}_{j}"], writes=[f"m_y{f}_{j}"])
            for j in range(NJ):
                t0 = tbase + j * 512
                zt = yacc[:, :, j * 512:(j + 1) * 512]
                zslot = f"m_yj{j}_"
                pend = None
                for f in range(KC):
                    epi.load_x(f, xT_in, t0, in_slot)
                    epi.chunk(f, s, zt[:, f, :], f"m_y{f}_{j}", zt, f"m_y", zsuffix=f"_{j}")
                    if pend is not None:
                        epi.stats_mm(pend, pst, pst2)
                    pend = f
                epi.stats_mm(pend, pst, pst2)
                epi.finish(s, pst, pst2, zt, "m_y", xT_out, t0, out_slot, zsuffix=f"_{j}")


def phase_ab_proj(P, C, xT_in, in_slot, w_in, qg, kvg, w_uq, w_ukv, ROPE, outs, s, T, TB=1024):
    nc = P.nc
    P.barrier()
    NJ = TB // 512
    QTa, KTa, Va, QTn, QTr, KTn, KTr, Vb = outs
    SCB = 192 ** -0.5
    with contextlib.ExitStack() as es:
        hTt = es.enter_context(nc.sbuf_tensor("ap_hT", [128, KC, TB], BF16))
        xs = [es.enter_context(nc.sbuf_tensor(f"ap_xs{i}", [128, 512], F32)) for i in range(2)]
        wb = [es.enter_context(nc.sbuf_tensor(f"ap_w{i}", [128, KC, 128], BF16)) for i in range(2)]
        wav = es.enter_context(nc.sbuf_tensor("ap_wav", [128, KC, 512], BF16))
        wkr = es.enter_context(nc.sbuf_tensor("ap_wkr", [128, KC, 2, 64], BF16))
        wuq = es.enter_context(nc.sbuf_tensor("ap_wuq", [128, 4, 768], BF16))
        wuqr = es.enter_context(nc.sbuf_tensor("ap_wuqr", [128, 4, 4, 64], BF16))
        wukv = es.enter_context(nc.sbuf_tensor("ap_wukv", [128, 2, 1024], BF16))
        gq = es.enter_context(nc.sbuf_tensor("ap_gq", [128, 4], F32))
        gkv = es.enter_context(nc.sbuf_tensor("ap_gkv", [128, 2], F32))
        ones512 = es.enter_context(nc.sbuf_tensor("ap_o512", [128, 128], BF16))
        ones256 = es.enter_context(nc.sbuf_tensor("ap_o256", [128, 128], BF16))
        cqg = es.enter_context(nc.sbuf_tensor("ap_cqg", [128, 4, TB], BF16))
        ckvg = es.enter_context(nc.sbuf_tensor("ap_ckvg", [128, 2, TB], BF16))
        sqkv = es.enter_context(nc.sbuf_tensor("ap_sqkv", [128, 2, TB], BF16))
        sq = [es.enter_context(nc.sbuf_tensor(f"ap_sq{i}", [128, 512], BF16)) for i in range(2)]
        rq = es.enter_context(nc.sbuf_tensor("ap_rq", [128, TB], F32))
        rkv = es.enter_context(nc.sbuf_tensor("ap_rkv", [128, TB], F32))
        rcol = es.enter_context(nc.sbuf_tensor("ap_rcol", [128, TB // 128], F32))
        rope = es.enter_context(nc.sbuf_tensor("ap_rope", [64, 2, TB], F32))
        st = [es.enter_context(nc.sbuf_tensor(f"ap_st{i}", [128, 512], BF16)) for i in range(2)]
        t1 = es.enter_context(nc.sbuf_tensor("ap_t1", [64, 512], F32))
        t2 = es.enter_context(nc.sbuf_tensor("ap_t2", [64, 512], F32))
        ps = [es.enter_context(nc.psum_tensor(f"ap_ps{i}", [128, 512], F32)) for i in range(2)]
        pr = [es.enter_context(nc.psum_tensor(f"ap_pr{i}", [128, 512], F32)) for i in range(2)]
        pstat = es.enter_context(nc.psum_tensor("ap_pstat", [128, 512], F32))
        pcol = es.enter_context(nc.psum_tensor("ap_pcol", [128, 512], F32))
        for q4 in range(4):
            P.op("pool", "dma_start", A(out=wav[:, :, q4 * 128:(q4 + 1) * 128],
                                        in_=w_in[:, 1024 + q4 * 128:1152 + q4 * 128].rearrange("(k p) f -> p k f", p=128)),
                 writes=["ap_wav"], dma="w2")
        P.op("pool", "dma_start", A(out=wkr[:, :, 0, :], in_=w_in[:, 2304:2368].rearrange("(k p) f -> p k f", p=128)),
             writes=["ap_wkr"], dma="w2")
        P.op("pool", "dma_start", A(out=wkr[:, :, 1, 0:32], in_=w_in[:, 2336:2368].rearrange("(k p) f -> p k f", p=128)),
             writes=["ap_wkr"], dma="w2")
        P.op("pool", "dma_start", A(out=wkr[:, :, 1, 32:64], in_=w_in[:, 2304:2336].rearrange("(k p) f -> p k f", p=128)),
             writes=["ap_wkr"], dma="w2")
        P.op("pool", "tensor_scalar", A(out=wkr[:, :, 1, 0:32], in0=wkr[:, :, 1, 0:32], scalar1=-1.0, scalar2=None, op0=ALU.mult),
             reads=["ap_wkr"], writes=["ap_wkr"])
        P.op("pool", "dma_start", A(out=wuq[:], in_=w_uq.rearrange("(k p) f -> p k f", p=128)), writes=["ap_wuq"], dma="w2")
        wuq4 = w_uq.rearrange("(k p) (h f) -> p k h f", p=128, h=4)
        for k in range(4):
            P.op("pool", "dma_start", A(out=wuqr[:, k, :, 0:32], in_=wuq4[:, k, :, 160:192]), writes=["ap_wuqr"], dma="w2")
            P.op("pool", "dma_start", A(out=wuqr[:, k, :, 32:64], in_=wuq4[:, k, :, 128:160]), writes=["ap_wuqr"], dma="w2")
        P.op("pool", "tensor_scalar", A(out=wuqr[:, :, :, 0:32], in0=wuqr[:, :, :, 0:32], scalar1=-1.0, scalar2=None, op0=ALU.mult),
             reads=["ap_wuqr"], writes=["ap_wuqr"])
        P.op("pool", "dma_start", A(out=wukv[:], in_=w_ukv.rearrange("(k p) f -> p k f", p=128)), writes=["ap_wukv"], dma="w2")
        P.op("sp", "dma_start", A(out=gq[:], in_=qg.rearrange("(k p) -> p k", p=128), allow_slow_non_contiguous=True),
             writes=["ap_gq"], dma="c")
        P.op("sp", "dma_start", A(out=gkv[:], in_=kvg.rearrange("(k p) -> p k", p=128), allow_slow_non_contiguous=True),
             writes=["ap_gkv"], dma="c")
        P.op("pool", "memset", A(ones512[:], 1.0 / 512), writes=["ap_o512"])
        P.op("pool", "memset", A(ones256[:], 1.0 / 256), writes=["ap_o256"])
        wcnt = 0
        n = 0

        def evac_store(par, dst, r0, nrows, c0, scale=None, mul_ap=None, mul_slot=None):
            if mul_ap is not None:
                P.op("dve", "tensor_tensor", A(out=st[par][0:nrows, :], in0=ps[par][0:nrows, :], in1=mul_ap, op=ALU.mult),
                     reads=[f"ap_ps{par}", mul_slot], writes=[f"ap_st{par}"])
            else:
                P.op("act", "activation", A(out=st[par][0:nrows, :], in_=ps[par][0:nrows, :], func=AF.Copy, scale=scale),
                     reads=[f"ap_ps{par}"], writes=[f"ap_st{par}"])
            P.op("sp", "dma_start", A(out=dst[r0:r0 + nrows, c0:c0 + 512], in_=st[par][0:nrows, :]),
                 reads=[f"ap_st{par}"], writes=["ab_proj"], dma=f"st{par}")

        def rstd_from(pstat_ap, dst_ap, dslot, extra_scale):
            P.op("dve", "tensor_scalar_add", A(out=dst_ap, in0=pstat_ap, scalar1=RMS_EPS), reads=["ap_pstat"], writes=[dslot])
            P.op("act", "activation", A(out=dst_ap, in_=dst_ap, func=AF.Sqrt), reads=[dslot], writes=[dslot])
            P.op("dve", "reciprocal", A(out=dst_ap, in_=dst_ap), reads=[dslot], writes=[dslot])
            if extra_scale != 1.0:
                P.op("dve", "tensor_scalar", A(out=dst_ap, in0=dst_ap, scalar1=extra_scale, scalar2=None, op0=ALU.mult),
                     reads=[dslot], writes=[dslot])

        for tb in range(T // TB):
            tbase = tb * TB
            hslots = load_hT(P, C, xT_in, in_slot, hTt, xs, s, tbase, TB, "ap_")
            P.op("sp", "dma_start", A(out=rope[:], in_=ROPE[:, :, tbase:tbase + TB]), writes=["ap_rope"], dma="c")
            for c in list(range(8)) + list(range(12, 18)):
                w = wb[wcnt % 2]
                wslot = f"ap_w{wcnt % 2}"
                wi = wcnt % 2
                wcnt += 1
                P.op("pool", "dma_start", A(out=w[:], in_=w_in[:, c * 128:(c + 1) * 128].rearrange("(k p) f -> p k f", p=128)),
                     writes=[wslot], dma=f"w{wi}")
                for j in range(NJ):
                    par = n % 2
                    n += 1
                    proj_fm(P, w_in, 0, 128, hTt, hslots, j, w, wslot, ps[par], f"ap_ps{par}")
                    c0 = tbase + j * 512
                    if c < 4:
                        evac_store(par, QTa, c * 128, 128, c0, scale=0.125)
                    elif c < 8:
                        evac_store(par, KTa, (c - 4) * 128, 128, c0, scale=1.0)
                    else:
                        isq = c < 16
                        kc = (c - 12) if isq else (c - 16)
                        dstg = cqg if isq else ckvg
                        gsl = gq if isq else gkv
                        P.op("act", "activation", A(out=dstg[:, kc, j * 512:(j + 1) * 512], in_=ps[par][:], func=AF.Copy,
                                                    scale=gsl[:, kc:kc + 1]),
                             reads=[f"ap_ps{par}", "ap_gq", "ap_gkv"], writes=[f"ap_lat{c}_{j}"])
                        sqt = sq[n % 2] if isq else None
                        if isq:
                            P.op("act", "activation", A(out=sqt[:], in_=ps[par][:], func=AF.Square),
                                 reads=[f"ap_ps{par}"], writes=[f"ap_sq{n % 2}"])
                            sq_ap, sq_slot = sqt[:], f"ap_sq{n % 2}"
                        else:
                            P.op("act", "activation", A(out=sqkv[:, kc, j * 512:(j + 1) * 512], in_=ps[par][:], func=AF.Square),
                                 reads=[f"ap_ps{par}"], writes=[f"ap_sqkv{kc}_{j}"])
                            sq_ap, sq_slot = sqkv[:, kc, j * 512:(j + 1) * 512], f"ap_sqkv{kc}_{j}"
                        nk = 4 if isq else 2
                        pst_t = pstat if j == 0 else pcol
                        pst_slot = "ap_pstat" if j == 0 else "ap_pcol"
                        _mm(P, pst_t[:], (ones512 if isq else ones256)[:], sq_ap, kc == 0, kc == nk - 1,
                            reads=[sq_slot, "ap_o512", "ap_o256"], writes=[pst_slot])
                        if kc == nk - 1:
                            dst = rq if isq else rkv
                            dslot = ("ap_rq" if isq else "ap_rkv") + f"_{j}"
                            dap = dst[:, j * 512:(j + 1) * 512]
                            P.op("dve", "tensor_scalar_add", A(out=dap, in0=pst_t[:], scalar1=RMS_EPS), reads=[pst_slot], writes=[dslot])
                            P.op("act", "activation", A(out=dap, in_=dap, func=AF.Sqrt), reads=[dslot], writes=[dslot])
                            P.op("dve", "reciprocal", A(out=dap, in_=dap), reads=[dslot], writes=[dslot])
                            if isq:
                                P.op("dve", "tensor_scalar", A(out=dap, in0=dap, scalar1=SCB, scalar2=None, op0=ALU.mult),
                                     reads=[dslot], writes=[dslot])
            for tt in range(TB // 128):
                par = n % 2
                n += 1
                for k in range(KC):
                    _mm(P, ps[par][:], hTt[:, k, tt * 128:(tt + 1) * 128], wav[:, k, :], k == 0, k == KC - 1,
                        reads=["ap_wav", hslots[k]], writes=[f"ap_ps{par}"])
                P.op("dve", "tensor_copy", A(out=st[par][:], in_=ps[par][:]), reads=[f"ap_ps{par}"], writes=[f"ap_st{par}"])
                P.op("sp", "dma_start", A(out=Va[tbase + tt * 128:tbase + (tt + 1) * 128, :], in_=st[par][:]),
                     reads=[f"ap_st{par}"], writes=["ab_proj"], dma=f"st{par}")
            for tt in range(TB // 128):
                j = tt // 4
                for kc in range(2):
                    _mm(P, pr[0][:, tt:tt + 1], sqkv[:, kc, tt * 128:(tt + 1) * 128], ones256[:, 0:1], tt == 0 and kc == 0, kc == 1,
                        reads=[f"ap_sqkv{kc}_{j}", "ap_o256"], writes=["ap_pr0"])
            NT = TB // 128
            P.op("dve", "tensor_scalar_add", A(out=rcol[:, 0:NT], in0=pr[0][:, 0:NT], scalar1=RMS_EPS), reads=["ap_pr0"], writes=["ap_rcol"])
            P.op("act", "activation", A(out=rcol[:, 0:NT], in_=rcol[:, 0:NT], func=AF.Sqrt), reads=["ap_rcol"], writes=["ap_rcol"])
            P.op("dve", "reciprocal", A(out=rcol[:, 0:NT], in_=rcol[:, 0:NT]), reads=["ap_rcol"], writes=["ap_rcol"])
            for j in range(NJ):
                c0 = tbase + j * 512
                for r in range(2):
                    for k in range(KC):
                        _mm(P, pr[r][0:64, :], wkr[:, k, r, :], hTt[:, k, j * 512:(j + 1) * 512], k == 0, k == KC - 1,
                            reads=["ap_wkr", hslots[k]], writes=[f"ap_pr{r}"])
                P.op("dve", "tensor_tensor", A(out=t1[:], in0=pr[0][0:64, :], in1=rope[:, 0, j * 512:(j + 1) * 512], op=ALU.mult),
                     reads=["ap_pr0", "ap_rope"], writes=["ap_t1"])
                P.op("dve", "tensor_tensor", A(out=t2[:], in0=pr[1][0:64, :], in1=rope[:, 1, j * 512:(j + 1) * 512], op=ALU.mult),
                     reads=["ap_pr1", "ap_rope"], writes=["ap_t2"])
                par = n % 2
                n += 1
                P.op("pool", "tensor_tensor", A(out=st[par][0:64, :], in0=t1[:], in1=t2[:], op=ALU.add),
                     reads=["ap_t1", "ap_t2"], writes=[f"ap_st{par}"])
                P.op("sp", "dma_start", A(out=KTr[0:64, c0:c0 + 512], in_=st[par][0:64, :]),
                     reads=[f"ap_st{par}"], writes=["ab_proj"], dma=f"st{par}")
            for j in range(NJ):
                c0 = tbase + j * 512
                lat_q = [f"ap_lat{12 + kc}_{j}" for kc in range(4)]
                lat_kv = [f"ap_lat{16 + kc}_{j}" for kc in range(2)]
                for h in range(4):
                    par = n % 2
                    n += 1
                    for kc in range(4):
                        _mm(P, ps[par][:], wuq[:, kc, h * 192:h * 192 + 128], cqg[:, kc, j * 512:(j + 1) * 512], kc == 0, kc == 3,
                            reads=["ap_wuq", lat_q[kc]], writes=[f"ap_ps{par}"])
                    evac_store(par, QTn, h * 128, 128, c0, mul_ap=rq[:, j * 512:(j + 1) * 512], mul_slot=f"ap_rq_{j}")
                    for kc in range(4):
                        _mm(P, pr[0][0:64, :], wuq[:, kc, h * 192 + 128:h * 192 + 192], cqg[:, kc, j * 512:(j + 1) * 512], kc == 0, kc == 3,
                            reads=["ap_wuq", lat_q[kc]], writes=["ap_pr0"])
                    for kc in range(4):
                        _mm(P, pr[1][0:64, :], wuqr[:, kc, h, :], cqg[:, kc, j * 512:(j + 1) * 512], kc == 0, kc == 3,
                            reads=["ap_wuqr", lat_q[kc]], writes=["ap_pr1"])
                    P.op("dve", "tensor_tensor", A(out=t1[:], in0=pr[0][0:64, :], in1=rope[:, 0, j * 512:(j + 1) * 512], op=ALU.mult),
                         reads=["ap_pr0", "ap_rope"], writes=["ap_t1"])
                    P.op("dve", "tensor_tensor", A(out=t2[:], in0=pr[1][0:64, :], in1=rope[:, 1, j * 512:(j + 1) * 512], op=ALU.mult),
                         reads=["ap_pr1", "ap_rope"], writes=["ap_t2"])
                    P.op("pool", "tensor_tensor", A(out=t1[:], in0=t1[:], in1=t2[:], op=ALU.add),
                         reads=["ap_t1", "ap_t2"], writes=["ap_t1"])
                    par = n % 2
                    n += 1
                    P.op("dve", "tensor_tensor", A(out=st[par][0:64, :], in0=t1[:], in1=rq[0:64, j * 512:(j + 1) * 512], op=ALU.mult),
                         reads=["ap_t1", f"ap_rq_{j}"], writes=[f"ap_st{par}"])
                    P.op("sp", "dma_start", A(out=QTr[h * 64:(h + 1) * 64, c0:c0 + 512], in_=st[par][0:64, :]),
                         reads=[f"ap_st{par}"], writes=["ab_proj"], dma=f"st{par}")
                for h in range(4):
                    par = n % 2
                    n += 1
                    for kc in range(2):
                        _mm(P, ps[par][:], wukv[:, kc, h * 256:h * 256 + 128], ckvg[:, kc, j * 512:(j + 1) * 512], kc == 0, kc == 1,
                            reads=["ap_wukv", lat_kv[kc]], writes=[f"ap_ps{par}"])
                    evac_store(par, KTn, h * 128, 128, c0, mul_ap=rkv[:, j * 512:(j + 1) * 512], mul_slot=f"ap_rkv_{j}")
            wv4 = wukv[:].rearrange("p k (h f) -> p k h f", h=4)
            for tt in range(TB // 128):
                j = tt // 4
                par = n % 2
                n += 1
                for kc in range(2):
                    _mm(P, ps[par][:], ckvg[:, kc, tt * 128:(tt + 1) * 128], wv4[:, kc, :, 128:256], kc == 0, kc == 1,
                        reads=["ap_wukv", f"ap_lat{16 + kc}_{j}"], writes=[f"ap_ps{par}"])
                P.op("dve", "tensor_scalar", A(out=st[par][:], in0=ps[par][:], scalar1=rcol[:, tt:tt + 1], scalar2=None, op0=ALU.mult),
                     reads=[f"ap_ps{par}", "ap_rcol"], writes=[f"ap_st{par}"])
                P.op("sp", "dma_start", A(out=Vb[tbase + tt * 128:tbase + (tt + 1) * 128, :], in_=st[par][:]),
                     reads=[f"ap_st{par}"], writes=["ab_proj"], dma=f"st{par}")


def phase_ab_attn(P, C, xT_in, in_slot, xT_out, out_slot, QTa, QTn, QTr, KTa_g, KTn_g, KTr_g, Va_g, Vb_g,
                  RT, DEL, lam, diff_g, w_out, s, layer, T):
    nc = P.nc
    P.barrier()
    NQC = T // 512
    NKT = 2 * T // 128
    HKT = T // 128
    OFF = 4 * (NQC - 1)
    lam_init = 0.8 - 0.6 * math.exp(-0.3 * layer)
    slopes = [2.0 ** (-8.0 * (h + 1) / 4) for h in range(4)]
    with contextlib.ExitStack() as es:
        kbuf = [es.enter_context(nc.sbuf_tensor(f"aa_k{i}", [128, 2 * T], BF16)) for i in range(2)]
        vbuf = [es.enter_context(nc.sbuf_tensor(f"aa_v{i}", [128, NKT, 128], BF16)) for i in range(2)]
        krp = es.enter_context(nc.sbuf_tensor("aa_krp", [64, 2 * T], BF16))
        qa = es.enter_context(nc.sbuf_tensor("aa_qa", [128, 4, 512], BF16))
        qn = es.enter_context(nc.sbuf_tensor("aa_qn", [128, 4, 512], BF16))
        qr = es.enter_context(nc.sbuf_tensor("aa_qr", [64, 4, 512], BF16))
        rt = es.enter_context(nc.sbuf_tensor("aa_rt", [128, 512], F32))
        dl = es.enter_context(nc.sbuf_tensor("aa_del", [128, 4, NKT + OFF + 1], F32))
        absd = [es.enter_context(nc.sbuf_tensor(f"aa_absd{i}", [128, 512], F32)) for i in range(2)]
        tmp = [es.enter_context(nc.sbuf_tensor(f"aa_tmp{i}", [128, 512], F32)) for i in range(2)]
        pT = [es.enter_context(nc.sbuf_tensor(f"aa_pT{i}", [128, 512], BF16)) for i in range(3)]
        onesk = es.enter_context(nc.sbuf_tensor("aa_ones", [128, 128], BF16))
        ones128 = es.enter_context(nc.sbuf_tensor("aa_o128", [128, 128], BF16))
        lamt = es.enter_context(nc.sbuf_tensor("aa_lam", [128, 4, 64], F32))
        lsc = es.enter_context(nc.sbuf_tensor("aa_lsc", [128, 8], F32))
        dg = es.enter_context(nc.sbuf_tensor("aa_dg", [128, 1], F32))
        rec = [es.enter_context(nc.sbuf_tensor(f"aa_rec{i}", [128, 512], F32)) for i in range(2)]
        o1 = es.enter_context(nc.sbuf_tensor("aa_o1", [128, 512], F32))
        dsq = es.enter_context(nc.sbuf_tensor("aa_dsq", [128, 512], BF16))
        ot = es.enter_context(nc.sbuf_tensor("aa_ot", [128, 8, 512], BF16))
        wo = [es.enter_context(nc.sbuf_tensor(f"aa_wo{i}", [128, 8, 128], BF16)) for i in range(2)]
        zt = es.enter_context(nc.sbuf_tensor("aa_z", [128, KC, 512], F32))
        psS = [es.enter_context(nc.psum_tensor(f"aa_ps{i}", [128, 512], F32)) for i in range(3)]
        pO = [es.enter_context(nc.psum_tensor(f"aa_pO{i}", [128, 512], F32)) for i in range(2)]
        pL = [es.enter_context(nc.psum_tensor(f"aa_pL{i}", [128, 512], F32)) for i in range(2)]
        prm = es.enter_context(nc.psum_tensor("aa_prm", [128, 512], F32))
        epi = Epi(P, C, es, "aa_")
        epi.pmean_slot, epi.pmsq_slot = "aa_ps1", "aa_ps2"
        P.op("sp", "dma_start", A(out=rt[:], in_=RT), writes=["aa_rt"], dma="c")
        P.op("sp", "dma_start", A(out=dl[:], in_=DEL), writes=["aa_del"], dma="c")
        P.op("sp", "dma_start", A(out=lamt[:], in_=lam.partition_broadcast(128)), writes=["aa_lam"], dma="c")
        P.op("sp", "dma_start", A(out=dg[:], in_=diff_g.rearrange("(p o) -> p o", o=1)), writes=["aa_dg"], dma="c")
        P.op("pool", "memset", A(onesk[:], 1.0), writes=["aa_ones"])
        P.op("pool", "memset", A(ones128[:], 1.0 / 128), writes=["aa_o128"])
        for half in range(2):
            P.op("sp", "dma_start", A(out=krp[:, half * T:(half + 1) * T], in_=KTr_g[half]), reads=["ab_g"], writes=["aa_krp"], dma="kv4")
        for i in range(2):
            P.op("dve", "tensor_tensor", A(out=lamt[:, 2 * i, :], in0=lamt[:, 2 * i, :], in1=lamt[:, 2 * i + 1, :], op=ALU.mult),
                 reads=["aa_lam"], writes=["aa_lam"])
            P.op("dve", "tensor_reduce", A(out=lsc[:, i:i + 1], in_=lamt[:, 2 * i, :], axis=mybir.AxisListType.X, op=ALU.add),
                 reads=["aa_lam"], writes=["aa_lsc"])
        P.op("act", "activation", A(out=lsc[:, 2:4], in_=lsc[:, 0:2], func=AF.Exp), reads=["aa_lsc"], writes=["aa_lsc"])
        P.op("dve", "tensor_tensor", A(out=lsc[:, 4:5], in0=lsc[:, 3:4], in1=lsc[:, 2:3], op=ALU.subtract),
             reads=["aa_lsc"], writes=["aa_lsc"])
        P.op("dve", "tensor_scalar_add", A(out=lsc[:, 4:5], in0=lsc[:, 4:5], scalar1=-lam_init), reads=["aa_lsc"], writes=["aa_lsc"])
        P.op("dve", "tensor_scalar", A(out=dg[:], in0=dg[:], scalar1=1.0 - lam_init, scalar2=None, op0=ALU.mult),
             reads=["aa_dg"], writes=["aa_dg"])
        kvc = 0
        si = 0
        wcnt = 0
        for qc in range(NQC):
            t0 = qc * 512
            P.op("sp", "dma_start", A(out=qa[:], in_=QTa[:, t0:t0 + 512].rearrange("(h p) t -> p h t", p=128)),
                 reads=["ab_proj"], writes=["aa_qa"], dma="kv5")
            P.op("sp", "dma_start", A(out=qn[:], in_=QTn[:, t0:t0 + 512].rearrange("(h p) t -> p h t", p=128)),
                 reads=["ab_proj"], writes=["aa_qn"], dma="kv6")
            P.op("sp", "dma_start", A(out=qr[:], in_=QTr[:, t0:t0 + 512].rearrange("(h p) t -> p h t", p=64)),
                 reads=["ab_proj"], writes=["aa_qr"], dma="kv7")
            for grp in range(2):
                for h in range(4):
                    kb, vb = kbuf[kvc % 2], vbuf[kvc % 2]
                    kslot, vslot = f"aa_k{kvc % 2}", f"aa_v{kvc % 2}"
                    ki = kvc % 2
                    kvc += 1
                    KT_g, V_g = (KTa_g, Va_g) if grp == 0 else (KTn_g, Vb_g)
                    for half in range(2):
                        P.op("sp", "dma_start", A(out=kb[:, half * T:(half + 1) * T], in_=KT_g[half, h * 128:(h + 1) * 128, :]),
                             reads=["ab_g"], writes=[kslot], dma=f"kv{ki}")
                        P.op("sp", "dma_start", A(out=vb[:, half * HKT:(half + 1) * HKT, :],
                                                  in_=V_g[half, :, h * 128:(h + 1) * 128].rearrange("(b p) f -> p b f", p=128)),
                             reads=["ab_g"], writes=[vslot], dma=f"kv{2 + ki}")
                    nmap = 2 if grp == 0 else 1
                    for kt in range(NKT):
                        ks = slice(kt * 128, (kt + 1) * 128)
                        if grp == 0:
                            ab = absd[kt % 2]
                            aslot = f"aa_absd{kt % 2}"
                            idx = kt - 4 * qc + OFF
                            P.op("act", "activation", A(out=ab[:], in_=rt[:], func=AF.Abs, scale=slopes[h],
                                                        bias=dl[:, h, idx:idx + 1]),
                                 reads=["aa_rt", "aa_del"], writes=[aslot])
                        for m in range(nmap):
                            i3 = si % 3
                            si += 1
                            if grp == 0:
                                rows = slice(64 * m, 64 * m + 64)
                                _mm(P, psS[i3][:], kb[rows, ks], qa[rows, h, :], True, True,
                                    reads=[kslot, "aa_qa"], writes=[f"aa_ps{i3}"])
                                tm = tmp[si % 2]
                                tslot = f"aa_tmp{si % 2}"
                                P.op("dve", "tensor_tensor", A(out=tm[:], in0=psS[i3][:], in1=ab[:], op=ALU.subtract),
                                     reads=[aslot, f"aa_ps{i3}"], writes=[tslot])
                                P.op("act", "activation", A(out=pT[i3][:], in_=tm[:], func=AF.Exp),
                                     reads=[tslot], writes=[f"aa_pT{i3}"])
                            else:
                                _mm(P, psS[i3][:], kb[:, ks], qn[:, h, :], True, False,
                                    reads=[kslot, "aa_qn"], writes=[f"aa_ps{i3}"])
                                _mm(P, psS[i3][:], krp[:, ks], qr[:, h, :], False, True,
                                    reads=["aa_krp", "aa_qr"], writes=[f"aa_ps{i3}"])
                                P.op("act", "activation", A(out=pT[i3][:], in_=psS[i3][:], func=AF.Exp),
                                     reads=[f"aa_ps{i3}"], writes=[f"aa_pT{i3}"])
                            _mm(P, pO[m][:], vb[:, kt, :], pT[i3][:], kt == 0, kt == NKT - 1,
                                reads=[vslot, f"aa_pT{i3}"], writes=[f"aa_pO{m}"])
                            _mm(P, pL[m][:], onesk[:], pT[i3][:], kt == 0, kt == NKT - 1,
                                reads=["aa_ones", f"aa_pT{i3}"], writes=[f"aa_pL{m}"])
                    for m in range(nmap):
                        P.op("dve", "reciprocal", A(out=rec[m][:], in_=pL[m][:]), reads=[f"aa_pL{m}"], writes=[f"aa_rec{m}"])
                    if grp == 1:
                        P.op("dve", "tensor_tensor", A(out=ot[:, 4 + h, :], in0=pO[0][:], in1=rec[0][:], op=ALU.mult),
                             reads=["aa_pO0", "aa_rec0"], writes=[f"aa_ot{4 + h}"])
                    else:
                        P.op("dve", "tensor_tensor", A(out=o1[:], in0=pO[0][:], in1=rec[0][:], op=ALU.mult),
                             reads=["aa_pO0", "aa_rec0"], writes=["aa_o1"])
                        P.op("dve", "tensor_tensor", A(out=rec[1][:], in0=pO[1][:], in1=rec[1][:], op=ALU.mult),
                             reads=["aa_pO1", "aa_rec1"], writes=["aa_rec1"])
                        P.op("dve", "scalar_tensor_tensor", A(out=o1[:], in0=rec[1][:], scalar=lsc[:, 4:5], in1=o1[:],
                                                              op0=ALU.mult, op1=ALU.add),
                             reads=["aa_rec1", "aa_lsc", "aa_o1"], writes=["aa_o1"])
                        P.op("act", "activation", A(out=dsq[:], in_=o1[:], func=AF.Square), reads=["aa_o1"], writes=["aa_dsq"])
                        _mm(P, prm[:], ones128[:], dsq[:], True, True, reads=["aa_o128", "aa_dsq"], writes=["aa_prm"])
                        P.op("dve", "tensor_scalar_add", A(out=rec[0][:], in0=prm[:], scalar1=RMS_EPS), reads=["aa_prm"], writes=["aa_rec0"])
                        P.op("act", "activation", A(out=rec[0][:], in_=rec[0][:], func=AF.Sqrt), reads=["aa_rec0"], writes=["aa_rec0"])
                        P.op("dve", "reciprocal", A(out=rec[0][:], in_=rec[0][:]), reads=["aa_rec0"], writes=["aa_rec0"])
                        P.op("dve", "tensor_tensor", A(out=o1[:], in0=o1[:], in1=rec[0][:], op=ALU.mult),
                             reads=["aa_o1", "aa_rec0"], writes=["aa_o1"])
                        P.op("act", "activation", A(out=ot[:, h, :], in_=o1[:], func=AF.Copy, scale=dg[:, 0:1]),
                             reads=["aa_o1", "aa_dg"], writes=[f"aa_ot{h}"])
            pend = None
            for f in range(KC):
                w = wo[wcnt % 2]
                wslot = f"aa_wo{wcnt % 2}"
                wi = wcnt % 2
                wcnt += 1
                P.op("pool", "dma_start", A(out=w[:], in_=w_out[:, f * 128:(f + 1) * 128].rearrange("(c p) f -> p c f", p=128)),
                     writes=[wslot], dma=f"w{wi}")
                epi.load_x(f, xT_in, t0, in_slot)
                for c in range(8):
                    _mm(P, psS[0][:], w[:, c, :], ot[:, c, :], c == 0, c == 7,
                        reads=[wslot, f"aa_ot{c}"], writes=["aa_ps0"])
                epi.chunk(f, s, psS[0][:], "aa_ps0", zt, "aa_z")
                if pend is not None:
                    epi.stats_mm(pend, psS[1], psS[2])
                pend = f
            epi.stats_mm(pend, psS[1], psS[2])
            epi.finish(s, psS[1], psS[2], zt, "aa_z", xT_out, t0, out_slot)


def ab_tables(T, half):
    NQC = T // 512
    NKT = 2 * T // 128
    OFF = 4 * (NQC - 1)
    rt = (np.arange(128)[:, None] - np.arange(512)[None, :]).astype(np.float32)
    idx = np.arange(NKT + OFF + 1)
    delta = ((idx - OFF) * 128 - half * T).astype(np.float32)
    slopes = np.array([2.0 ** (-8.0 * (h + 1) / 4) for h in range(4)], np.float32)
    dl = np.broadcast_to((slopes[:, None] * delta[None, :])[None], (128, 4, NKT + OFF + 1))
    return rt, np.ascontiguousarray(dl.astype(np.float32))


def rope_table(T, half):
    inv = (10000.0 ** (-np.arange(0, 64, 2, dtype=np.float32) / 64)).astype(np.float32)
    pos = np.arange(half * T, (half + 1) * T, dtype=np.float32)
    ang = (pos[None, :] * inv[:, None]).astype(np.float32)
    out = np.zeros((64, 2, T), np.float32)
    out[:32, 0] = np.cos(ang); out[32:, 0] = np.cos(ang)
    out[:32, 1] = np.sin(ang); out[32:, 1] = np.sin(ang)
    return out


PAIRS = [[0, 1], [2, 3], [4, 5], [6, 7]]
AB_OUTS = [("QTa", 512, None), ("KTa", 512, None), ("Va", None, 512), ("QTn", 512, None), ("QTr", 256, None),
           ("KTn", 512, None), ("KTr", 64, None), ("Vb", None, 512)]


class Builder:
    def __init__(self, T):
        self.T = T
        self.nc = bass.Bass("TRN2", target_bir_lowering=False)
        self.P = Prog(self.nc)
        self.C = Ctx()
        self.ext_in = {}
        self.ext_out = []
        self.tens = {}

    def inp(self, name, shape, dtype, src):
        if name not in self.tens:
            self.tens[name] = self.nc.dram_tensor(name, list(shape), dtype, kind="ExternalInput").ap()
            self.ext_in[name] = src
        return self.tens[name]

    def out(self, name, shape, dtype):
        if name not in self.tens:
            self.tens[name] = self.nc.dram_tensor(name, list(shape), dtype, kind="ExternalOutput").ap()
            self.ext_out.append(name)
        return self.tens[name]

    def scratch(self, name, shape, dtype):
        if name not in self.tens:
            self.tens[name] = self.nc.dram_tensor(name, list(shape), dtype, kind="Internal").ap()
        return self.tens[name]


def build_launch(stages, T, mode, first_x_external=True, last=False):
    B = Builder(T)
    nc, P, C = B.nc, B.P, B.C
    with contextlib.ExitStack() as es:
        C.es = es
        C.ident_dram = B.inp("ident", [128, 128], F32, ("const", "ident"))
        subs = []
        for st in stages:
            if st[0] in ("ab_proj", "c_proj", "ab_attn", "c_attn"):
                subs.append(2 * st[1])
            elif st[0] in ("ffn", "moe"):
                subs.append(2 * st[1] + 1)
        subs = sorted(set(subs))
        adaw = {}
        for l in sorted(set(s // 2 for s in subs)):
            adaw[l] = B.inp(f"ada_w{l}", [2, D, 3 * D], F32, ("ada_w", l))
        ada_b = B.inp("ada_b", [4, 2, 3 * D], F32, ("ada_b",))
        ln_g = B.inp("ln_g", [4, 2, D], F32, ("ln_g",))
        ln_b = B.inp("ln_b", [4, 2, D], F32, ("ln_b",))
        c_in = B.inp("c", [D], F32, ("c_core",))
        phase_prologue(P, C, c_in, adaw, ada_b, ln_g, ln_b, subs)
        x_cur = B.inp("xT_in", [D, T], F32, ("x_core",))
        x_slot = "xT_in"
        n_sub = sum(1 for st in stages if st[0] in ("ab_attn", "c_attn", "ffn", "moe"))
        done = 0
        pp = 0

        def next_x():
            nonlocal done, pp
            done += 1
            if done == n_sub:
                return B.out("xT_out", [D, T], F32), "xT_out"
            pp += 1
            nm = f"xs{pp % 2}"
            return B.scratch(nm, [D, T], F32), nm

        for st in stages:
            kind = st[0]
            if kind == "ab_proj":
                l = st[1]; i = l // 2
                outs = []
                for nm, rows, cols in AB_OUTS:
                    shp = [rows, T] if rows is not None else [T, cols]
                    outs.append(B.out(nm, shp, BF16) if mode == "host" else B.scratch(nm, shp, BF16))
                phase_ab_proj(P, C, x_cur, x_slot,
                              B.inp(f"ab_w_in{i}", [D, 2368], F32, ("ab_w_in", i)),
                              B.inp(f"ab_qg{i}", [512], F32, ("ab_q_norm_g", i)),
                              B.inp(f"ab_kvg{i}", [256], F32, ("ab_kv_norm_g", i)),
                              B.inp(f"ab_w_uq{i}", [512, 768], F32, ("ab_w_uq", i)),
                              B.inp(f"ab_w_ukv{i}", [256, 1024], F32, ("ab_w_ukv", i)),
                              B.inp("ROPE", [64, 2, T], F32, ("const", "rope")), outs, 2 * l, T)
            elif kind == "xchg_ab":
                P.barrier()
                for nm, rows, cols in AB_OUTS:
                    if nm.startswith("Q"):
                        continue
                    shp = [rows, T] if rows is not None else [T, cols]
                    src = B.tens[nm]
                    dst = B.scratch(nm + "_g", [2 * shp[0], shp[1]], BF16)
                    P.op("pool", "collective_compute", A("AllGather", ALU.bypass, replica_groups=PAIRS, ins=[src], outs=[dst]),
                         reads=["ab_proj"], writes=["ab_g"], dma="cc")
            elif kind == "ab_attn":
                l = st[1]; i = l // 2
                g = {}
                for nm, rows, cols in AB_OUTS:
                    shp = [rows, T] if rows is not None else [T, cols]
                    if nm.startswith("Q"):
                        g[nm] = B.inp(nm, shp, BF16, ("prev", nm)) if mode == "host" else B.tens[nm]
                    else:
                        if mode == "host":
                            g[nm] = B.inp(nm + "_g", [2] + shp, BF16, ("pair", nm))
                        else:
                            g[nm] = B.tens[nm + "_g"].rearrange("(r a) b -> r a b", r=2)
                NI = 2 * T // 128 + 4 * (T // 512 - 1) + 1
                xo, xo_slot = next_x()
                phase_ab_attn(P, C, x_cur, x_slot, xo, xo_slot, g["QTa"], g["QTn"], g["QTr"], g["KTa"], g["KTn"], g["KTr"],
                              g["Va"], g["Vb"], B.inp("RT", [128, 512], F32, ("const", "rt")),
                              B.inp("DEL", [128, 4, NI], F32, ("const", "del")),
                              B.inp(f"ab_lam{i}", [256], F32, ("ab_lam", i)), B.inp(f"ab_dg{i}", [128], F32, ("ab_diff_g", i)),
                              B.inp(f"ab_w_out{i}", [1024, D], F32, ("ab_w_out", i)), 2 * l, l, T)
                x_cur, x_slot = xo, xo_slot
            elif kind == "ffn":
                l = st[1]; i = l // 2
                xo, xo_slot = next_x()
                phase_ffn(P, C, x_cur, x_slot, xo, xo_slot, B.inp(f"ffn_wg{i}", [D, D_FF], F32, ("ffn_w_gate", i)),
                          B.inp(f"ffn_wu{i}", [D, D_FF], F32, ("ffn_w_up", i)),
                          B.inp(f"ffn_wd{i}", [D_FF, D], F32, ("ffn_w_down", i)), 2 * l + 1, T)
                x_cur, x_slot = xo, xo_slot
            elif kind == "c_proj":
                l = st[1]; i = l // 2
                mk = (lambda n, sh: B.out(n, sh, BF16)) if mode == "host" else (lambda n, sh: B.scratch(n, sh, BF16))
                QT, KT, V = mk("cQT", [2048, T]), mk("cKT", [512, T]), mk("cV", [T, 512])
                phase_c_proj(P, C, x_cur, x_slot, B.inp(f"c_w_in{i}", [D, 3072], F32, ("c_w_in", i)), QT, KT, V, 2 * l, T)
            elif kind == "xchg_c":
                P.barrier()
                KT, V = B.tens["cKT"], B.tens["cV"]
                kp = B.scratch("cKTp", [2 * 512, 128], BF16)
                vp = B.scratch("cVp", [2 * 128, 512], BF16)
                P.op("sp", "dma_start", A(out=kp[0:512, :], in_=KT[:, 0:128]), reads=["c_qkv"], writes=["c_pk"], dma="kv0")
                P.op("sp", "dma_start", A(out=kp[512:1024, :], in_=KT[:, T - 128:T]), reads=["c_qkv"], writes=["c_pk"], dma="kv1")
                P.op("sp", "dma_start", A(out=vp[0:128, :], in_=V[0:128, :]), reads=["c_qkv"], writes=["c_pk"], dma="kv2")
                P.op("sp", "dma_start", A(out=vp[128:256, :], in_=V[T - 128:T, :]), reads=["c_qkv"], writes=["c_pk"], dma="kv3")
                kg = B.scratch("cKTg", [4 * 512, 128], BF16)
                vg = B.scratch("cVg", [4 * 128, 512], BF16)
                P.op("pool", "collective_compute", A("AllGather", ALU.bypass, replica_groups=PAIRS, ins=[kp], outs=[kg]),
                     reads=["c_pk"], writes=["c_halo"], dma="cc")
                P.op("pool", "collective_compute", A("AllGather", ALU.bypass, replica_groups=PAIRS, ins=[vp], outs=[vg]),
                     reads=["c_pk"], writes=["c_halo"], dma="cc")
            elif kind == "c_attn":
                l = st[1]; i = l // 2
                if mode == "host":
                    QT = B.inp("cQT", [2048, T], BF16, ("prev", "cQT"))
                    KT = B.inp("cKT", [512, T], BF16, ("prev", "cKT"))
                    V = B.inp("cV", [T, 512], BF16, ("prev", "cV"))
                    KTh = B.inp("cKTh", [2, 512, 128], BF16, ("halo", "cKT"))
                    Vh = B.inp("cVh", [2, 128, 512], BF16, ("halo", "cV"))
                else:
                    QT, KT, V = B.tens["cQT"], B.tens["cKT"], B.tens["cV"]
                    KTh = B.tens["cKTg"].rearrange("(r a) b -> r a b", r=4)[1:3]
                    Vh = B.tens["cVg"].rearrange("(r a) b -> r a b", r=4)[1:3]
                xo, xo_slot = next_x()
                phase_c_attn(P, C, x_cur, x_slot, xo, xo_slot, QT, KT, V, KTh, Vh,
                             B.inp("HB", [128, 2], F32, ("const", "hb")), B.inp("CT", [128, 384], F32, ("const", "ct")),
                             B.inp(f"c_sink{i}", [16], F32, ("c_sink", i)), B.inp(f"c_w_out{i}", [D, D], F32, ("c_w_out", i)), 2 * l, T)
                x_cur, x_slot = xo, xo_slot
            elif kind == "moe":
                l = st[1]; i = l // 2
                xo, xo_slot = next_x()
                phase_moe(P, C, x_cur, x_slot, xo, xo_slot, B.inp(f"moe_wr{i}", [D, N_EXP], F32, ("moe_w_router", i)),
                          B.inp(f"moe_br{i}", [N_EXP], F32, ("moe_b_router", i)),
                          B.inp(f"moe_wg{i}", [N_EXP, D, E_FF], F32, ("moe_w_gate", i)),
                          B.inp(f"moe_wu{i}", [N_EXP, D, E_FF], F32, ("moe_w_up", i)),
                          B.inp(f"moe_wd{i}", [N_EXP, E_FF, D], F32, ("moe_w_down", i)), 2 * l + 1, T)
                x_cur, x_slot = xo, xo_slot
        P.wait_all_dma("sp", None)
        P.emit()
    return B


def _run(B, inputs, consts, x_cores, prev, ncores, T):
    in_maps = []
    for core in range(ncores):
        b, half = divmod(core, 2)
        m = {}
        for name, src in B.ext_in.items():
            k = src[0]
            if k == "const":
                v = consts[src[1]][half] if isinstance(consts[src[1]], list) else consts[src[1]]
            elif k == "c_core":
                v = inputs["c"][b]
            elif k == "x_core":
                v = x_cores[core]
            elif k == "prev":
                v = prev[core][src[1]]
            elif k == "pair":
                v = np.stack([prev[2 * b][src[1]], prev[2 * b + 1][src[1]]])
            elif k == "halo":
                a0 = prev[core][src[1]]
                if src[1] == "cKT":
                    v = np.zeros((2, 512, 128), a0.dtype)
                    if half == 1:
                        v[0] = prev[core - 1]["cKT"][:, -128:]
                    else:
                        v[1] = prev[core + 1]["cKT"][:, :128]
                else:
                    v = np.zeros((2, 128, 512), a0.dtype)
                    if half == 1:
                        v[0] = prev[core - 1]["cV"][-128:]
                    else:
                        v[1] = prev[core + 1]["cV"][:128]
            elif len(src) == 2:
                v = inputs[k][src[1]]
            else:
                v = inputs[k]
            if name.startswith("ab_lam"):
                v = np.ascontiguousarray(v).reshape(-1)
            m[name] = np.ascontiguousarray(v)
        in_maps.append(m)
    res = run_bass_kernel_spmd(B.nc, in_maps, core_ids=list(range(ncores)))
    return res.results


def make_consts(T):
    consts = {"ident": np.eye(128, dtype=np.float32), "ct": c_tables()}
    consts["rope"] = [rope_table(T, h) for h in range(2)]
    tabs = [ab_tables(T, h) for h in range(2)]
    consts["rt"] = tabs[0][0]
    consts["del"] = [tabs[h][1] for h in range(2)]
    hb = []
    for h in range(2):
        a = np.zeros((128, 2), np.float32)
        a[:, 0] = 0.0 if h == 1 else -30000.0
        a[:, 1] = 0.0 if h == 0 else -30000.0
        hb.append(a)
    consts["hb"] = hb
    return consts


LAUNCHES_HOST = [
    [("ab_proj", 0)],
    [("ab_attn", 0), ("ffn", 0), ("c_proj", 1)],
    [("c_attn", 1), ("moe", 1), ("ab_proj", 2)],
    [("ab_attn", 2), ("ffn", 2), ("c_proj", 3)],
    [("c_attn", 3), ("moe", 3)],
]
STAGES_FUSED = [("ab_proj", 0), ("xchg_ab",), ("ab_attn", 0), ("ffn", 0), ("c_proj", 1), ("xchg_c",), ("c_attn", 1), ("moe", 1),
                ("ab_proj", 2), ("xchg_ab",), ("ab_attn", 2), ("ffn", 2), ("c_proj", 3), ("xchg_c",), ("c_attn", 3), ("moe", 3)]
MODE = "host"


def kernel(**inputs):
    inputs = {k: np.asarray(v) for k, v in inputs.items()}
    x = inputs["x"]
    Bsz, S, _ = x.shape
    T = S // 2
    ncores = 2 * Bsz
    consts = make_consts(T)
    x_cores = [np.ascontiguousarray(x[c // 2, (c % 2) * T:(c % 2 + 1) * T, :].T) for c in range(ncores)]
    prev = [dict() for _ in range(ncores)]
    if MODE == "host":
        for stages in LAUNCHES_HOST:
            Bd = build_launch(stages, T, "host")
            res = _run(Bd, inputs, consts, x_cores, prev, ncores, T)
            for c in range(ncores):
                for nm in Bd.ext_out:
                    if nm == "xT_out":
                        x_cores[c] = res[c][nm]
                    else:
                        prev[c][nm] = res[c][nm]
    else:
        Bd = build_launch(STAGES_FUSED, T, "cc")
        res = _run(Bd, inputs, consts, x_cores, prev, ncores, T)
        for c in range(ncores):
            x_cores[c] = res[c]["xT_out"]
    out = np.empty((Bsz, S, D), np.float32)
    for c in range(ncores):
        out[c // 2, (c % 2) * T:(c % 2 + 1) * T, :] = x_cores[c].T
    return out
```
